# Optimizing a Trainium2 kernel written in Bass

```python
import math
import jax, jax.numpy as jnp
from jax import lax
import numpy as np

D_MODEL = 2048
BATCH = 4
SEQ = 4096
DEPTH = 4

N_META = 16
EPS = 1e-6
ML_HEADS = 8
ML_DV = D_MODEL // ML_HEADS
ML_DQK = ML_DV // 2
ML_CHUNK = 64
GATE_CAP = 15.0
ML_QK_W = ML_HEADS * ML_DQK
ML_V_W = ML_HEADS * ML_DV
ML_IN = 2 * ML_QK_W + 2 * ML_V_W + 2 * ML_HEADS
ML_SPLITS = (ML_QK_W, 2 * ML_QK_W, 2 * ML_QK_W + ML_V_W,
             2 * ML_QK_W + 2 * ML_V_W, 2 * ML_QK_W + 2 * ML_V_W + ML_HEADS)
ML_PAD = (-N_META) % ML_CHUNK
NEG_BIG = -1e30
FOX_HEADS = 32
FOX_DH = D_MODEL // FOX_HEADS
FOX_IN = 4 * D_MODEL + FOX_HEADS
FOX_SPLITS = (D_MODEL, 2 * D_MODEL, 3 * D_MODEL, 4 * D_MODEL)
Q_BLOCK = 128
D_FF = 11 * D_MODEL // 4
N_EXPERTS = 8
TOP_K = 2
D_FF_EXPERT = 11 * D_MODEL // 4

kernel_name = "hybrid_mlstm_fox_moe_trunk"


def rmsnorm(x, g):
    xf = x.astype(jnp.float32)
    y = xf * lax.rsqrt(jnp.mean(xf * xf, axis=-1, keepdims=True) + EPS)
    return (y * g.astype(jnp.float32)).astype(x.dtype)


def softcap(z):
    return GATE_CAP * jnp.tanh(z / GATE_CAP)


def swiglu(h, w_gate, w_up, w_down):
    return (jax.nn.silu(h @ w_gate) * (h @ w_up)) @ w_down


def mlstm_mixer(h, w_in, b_i, b_f, h_gain, w_out):
    B, T, _ = h.shape
    f32 = jnp.float32
    H, L = ML_HEADS, ML_CHUNK
    q, k, v, o, ig, fg = jnp.split(h @ w_in, ML_SPLITS, axis=-1)

    def heads(t, d):
        t = t.reshape(B, T, H, d).transpose(0, 2, 1, 3).astype(f32)
        return jnp.pad(t, ((0, 0), (0, 0), (ML_PAD, 0), (0, 0)))

    q = heads(q, ML_DQK)
    k = heads(k, ML_DQK) * (ML_DQK ** -0.5)
    v = heads(v, ML_DV)
    li = softcap(ig.astype(f32) + b_i.astype(f32)).transpose(0, 2, 1)
    lf = jax.nn.log_sigmoid(softcap(fg.astype(f32) + b_f.astype(f32))).transpose(0, 2, 1)
    li = jnp.pad(li, ((0, 0), (0, 0), (ML_PAD, 0)), constant_values=NEG_BIG)
    lf = jnp.pad(lf, ((0, 0), (0, 0), (ML_PAD, 0)))
    Tp = T + ML_PAD
    NC = Tp // L
    q = q.reshape(B, H, NC, L, ML_DQK)
    k = k.reshape(B, H, NC, L, ML_DQK)
    v = v.reshape(B, H, NC, L, ML_DV)
    li = li.reshape(B, H, NC, L)
    lf = lf.reshape(B, H, NC, L)

    b = jnp.cumsum(lf, axis=-1)
    g = b[..., -1]
    a = g[..., None] - b + li
    m_loc = jnp.max(a, axis=-1)
    w_loc = jnp.exp(a - m_loc[..., None])
    C_loc = jnp.einsum('bhcs,bhcsk,bhcsv->bhckv', w_loc, k, v)
    n_loc = jnp.einsum('bhcs,bhcsk->bhck', w_loc, k)

    def step(carry, inp):
        C, n, m = carry
        Cl, nl, ml, gl = inp
        m_new = jnp.maximum(gl + m, ml)
        sp = jnp.exp(gl + m - m_new)
        sl = jnp.exp(ml - m_new)
        C_new = sp[..., None, None] * C + sl[..., None, None] * Cl
        n_new = sp[..., None] * n + sl[..., None] * nl
        return (C_new, n_new, m_new), (C, n, m)

    init = (jnp.zeros((B, H, ML_DQK, ML_DV), f32), jnp.zeros((B, H, ML_DQK), f32), jnp.zeros((B, H), f32))
    xs = (jnp.moveaxis(C_loc, 2, 0), jnp.moveaxis(n_loc, 2, 0), jnp.moveaxis(m_loc, 2, 0), jnp.moveaxis(g, 2, 0))
    _, (C_prev, n_prev, m_prev) = lax.scan(step, init, xs)
    C_prev = jnp.moveaxis(C_prev, 0, 2)
    n_prev = jnp.moveaxis(n_prev, 0, 2)
    m_prev = jnp.moveaxis(m_prev, 0, 2)

    causal = jnp.tril(jnp.ones((L, L), dtype=bool))
    d_log = jnp.where(causal, b[..., :, None] - b[..., None, :] + li[..., None, :], -jnp.inf)
    inter = b + m_prev[..., None]
    m_t = jnp.maximum(inter, jnp.max(d_log, axis=-1))
    s = jnp.einsum('bhctk,bhcsk->bhcts', q, k) * jnp.exp(d_log - m_t[..., None])
    w_inter = jnp.exp(inter - m_t)
    num = jnp.einsum('bhcts,bhcsv->bhctv', s, v) + w_inter[..., None] * jnp.einsum('bhctk,bhckv->bhctv', q, C_prev)
    den = jnp.sum(s, axis=-1) + w_inter * jnp.einsum('bhctk,bhck->bhct', q, n_prev)
    hh = num / jnp.maximum(jnp.abs(den), jnp.exp(-m_t))[..., None]
    hh = hh.reshape(B, H, Tp, ML_DV)[:, :, ML_PAD:]
    hh = rmsnorm(hh, h_gain.reshape(H, 1, ML_DV))
    hh = hh.transpose(0, 2, 1, 3).reshape(B, T, ML_V_W).astype(h.dtype)
    return (jax.nn.sigmoid(o) * hh) @ w_out


def fox_mixer(h, w_in, b_f, q_gain, k_gain, w_out):
    B, T, _ = h.shape
    f32 = jnp.float32
    H, dh = FOX_HEADS, FOX_DH
    q, k, v, og, fg = jnp.split(h @ w_in, FOX_SPLITS, axis=-1)

    def heads(t):
        return t.reshape(B, T, H, dh).transpose(0, 2, 1, 3)

    q = rmsnorm(heads(q), q_gain) * (dh ** -0.5)
    k = rmsnorm(heads(k), k_gain)
    v = heads(v)
    c = jnp.cumsum(jax.nn.log_sigmoid(fg.astype(f32) + b_f.astype(f32)), axis=1).transpose(0, 2, 1)
    pos = jnp.arange(T)

    def attend(q_blk, c_blk, t_idx):
        logits = jnp.einsum('bhtd,bhsd->bhts', q_blk, k).astype(f32) + (c_blk[..., :, None] - c[..., None, :])
        logits = jnp.where(t_idx[:, None] >= pos[None, :], logits, -jnp.inf)
        p = jax.nn.softmax(logits, axis=-1)
        return jnp.einsum('bhts,bhsd->bhtd', p.astype(v.dtype), v)

    y_meta = attend(q[:, :, :N_META], c[:, :, :N_META], pos[:N_META])
    nb = (T - N_META) // Q_BLOCK
    q_blocks = q[:, :, N_META:].reshape(B, H, nb, Q_BLOCK, dh).transpose(2, 0, 1, 3, 4)
    c_blocks = c[:, :, N_META:].reshape(B, H, nb, Q_BLOCK).transpose(2, 0, 1, 3)
    t_blocks = (N_META + jnp.arange(nb * Q_BLOCK)).reshape(nb, Q_BLOCK)
    y_real = lax.map(lambda args: attend(*args), (q_blocks, c_blocks, t_blocks))
    y_real = y_real.transpose(1, 2, 0, 3, 4).reshape(B, H, T - N_META, dh)
    y = jnp.concatenate([y_meta, y_real], axis=2).transpose(0, 2, 1, 3).reshape(B, T, D_MODEL)
    return (jax.nn.sigmoid(og) * y) @ w_out


def moe_ffn(h, router, w_gate, w_up, w_down):
    B, T, D = h.shape
    hf = h.reshape(B * T, D)
    logits = (hf @ router).astype(jnp.float32)
    top_v, top_i = lax.top_k(logits, TOP_K)
    gates = jax.nn.softmax(top_v, axis=-1)
    combine = jnp.einsum('nk,nke->ne', gates, jax.nn.one_hot(top_i, N_EXPERTS, dtype=jnp.float32)).astype(h.dtype)
    y = jnp.zeros_like(hf)
    for e in range(N_EXPERTS):
        y = y + combine[:, e:e + 1] * swiglu(hf, w_gate[e], w_up[e], w_down[e])
    return y.reshape(B, T, D)


def setup_inputs(seed: int = 0) -> dict:
    key = jax.random.key(seed)
    ks = jax.random.split(key, 32)
    f32 = jnp.float32
    D = D_MODEL
    n_even = (DEPTH + 1) // 2
    n_odd = DEPTH // 2
    out_scale = (2.0 * DEPTH) ** -0.5

    def nrm(k, shape, fan_in, scale=1.0):
        return jax.random.normal(k, shape, f32) * (fan_in ** -0.5) * scale

    def gain(k, shape):
        return 1.0 + 0.02 * jax.random.normal(k, shape, f32)

    x = jax.random.normal(ks[0], (BATCH, SEQ, D), f32)
    meta_tokens = jax.random.normal(ks[1], (N_META, D), f32)
    ml_norm = gain(ks[2], (n_even, D))
    ml_w_in = nrm(ks[3], (n_even, D, ML_IN), D)
    ml_b_i = 0.1 * jax.random.normal(ks[4], (n_even, ML_HEADS), f32)
    ml_b_f = jnp.linspace(3.0, 6.0, ML_HEADS, dtype=f32)[None, :] + 0.1 * jax.random.normal(ks[5], (n_even, ML_HEADS), f32)
    ml_h_gain = gain(ks[6], (n_even, ML_V_W))
    ml_w_out = nrm(ks[7], (n_even, ML_V_W, D), ML_V_W, out_scale)
    ffn_norm = gain(ks[8], (n_even, D))
    ffn_w_gate = nrm(ks[9], (n_even, D, D_FF), D)
    ffn_w_up = nrm(ks[10], (n_even, D, D_FF), D)
    ffn_w_down = nrm(ks[11], (n_even, D_FF, D), D_FF, out_scale)
    fox_norm = gain(ks[12], (n_odd, D))
    fox_w_in = nrm(ks[13], (n_odd, D, FOX_IN), D)
    fox_b_f = jnp.linspace(1.0, 5.0, FOX_HEADS, dtype=f32)[None, :] + 0.1 * jax.random.normal(ks[14], (n_odd, FOX_HEADS), f32)
    fox_q_gain = gain(ks[15], (n_odd, FOX_DH))
    fox_k_gain = gain(ks[16], (n_odd, FOX_DH))
    fox_w_out = nrm(ks[17], (n_odd, D, D), D, out_scale)
    moe_norm = gain(ks[18], (n_odd, D))
    moe_router = nrm(ks[19], (n_odd, D, N_EXPERTS), D)
    moe_w_gate = nrm(ks[20], (n_odd, N_EXPERTS, D, D_FF_EXPERT), D)
    moe_w_up = nrm(ks[21], (n_odd, N_EXPERTS, D, D_FF_EXPERT), D)
    moe_w_down = nrm(ks[22], (n_odd, N_EXPERTS, D_FF_EXPERT, D), D_FF_EXPERT, out_scale)
    final_norm = gain(ks[23], (D,))
    return {"x": x, "meta_tokens": meta_tokens,
            "ml_norm": ml_norm, "ml_w_in": ml_w_in, "ml_b_i": ml_b_i, "ml_b_f": ml_b_f,
            "ml_h_gain": ml_h_gain, "ml_w_out": ml_w_out,
            "ffn_norm": ffn_norm, "ffn_w_gate": ffn_w_gate, "ffn_w_up": ffn_w_up, "ffn_w_down": ffn_w_down,
            "fox_norm": fox_norm, "fox_w_in": fox_w_in, "fox_b_f": fox_b_f,
            "fox_q_gain": fox_q_gain, "fox_k_gain": fox_k_gain, "fox_w_out": fox_w_out,
            "moe_norm": moe_norm, "moe_router": moe_router, "moe_w_gate": moe_w_gate,
            "moe_w_up": moe_w_up, "moe_w_down": moe_w_down,
            "final_norm": final_norm}


def reference(x, meta_tokens,
              ml_norm, ml_w_in, ml_b_i, ml_b_f, ml_h_gain, ml_w_out,
              ffn_norm, ffn_w_gate, ffn_w_up, ffn_w_down,
              fox_norm, fox_w_in, fox_b_f, fox_q_gain, fox_k_gain, fox_w_out,
              moe_norm, moe_router, moe_w_gate, moe_w_up, moe_w_down,
              final_norm):
    B = x.shape[0]
    meta = jnp.broadcast_to(meta_tokens[None].astype(x.dtype), (B, N_META, D_MODEL))
    h = jnp.concatenate([meta, x], axis=1)
    for i in range(DEPTH):
        j = i // 2
        if i % 2 == 0:
            h = h + mlstm_mixer(rmsnorm(h, ml_norm[j]), ml_w_in[j], ml_b_i[j], ml_b_f[j], ml_h_gain[j], ml_w_out[j])
            h = h + swiglu(rmsnorm(h, ffn_norm[j]), ffn_w_gate[j], ffn_w_up[j], ffn_w_down[j])
        else:
            h = h + fox_mixer(rmsnorm(h, fox_norm[j]), fox_w_in[j], fox_b_f[j], fox_q_gain[j], fox_k_gain[j], fox_w_out[j])
            h = h + moe_ffn(rmsnorm(h, moe_norm[j]), moe_router[j], moe_w_gate[j], moe_w_up[j], moe_w_down[j])
    return rmsnorm(h[:, N_META:], final_norm)
```

```python
import numpy as np
import concourse.bass as bass
import concourse.mybir as mybir
from concourse.bass_utils import run_bass_kernel_spmd

F32 = mybir.dt.float32
BF16 = mybir.dt.bfloat16
AF = mybir.ActivationFunctionType
ALU = mybir.AluOpType
AX = mybir.AxisListType

D = 2048
KC = 16
N_META = 16
EPS = 1e-6
ML_H = 8
ML_DQK = 128
ML_DV = 256
ML_IN = 6160
FOX_H = 32
FOX_DH = 64
FOX_IN = 8224
D_FF = 5632
FC = 44
N_EXP = 8
GATE_CAP = 15.0


class _Op:
    __slots__ = ("eng", "idx", "fn", "deps", "signal", "is_dma", "sem", "val", "pre", "phase")

    def __init__(self, eng, idx, fn, is_dma):
        self.eng = eng
        self.idx = idx
        self.fn = fn
        self.deps = []
        self.signal = False
        self.is_dma = is_dma
        self.sem = None
        self.val = None
        self.pre = None


class Prog:
    COMPUTE = ("pe", "act", "dve", "pool")
    QUEUES = ("sp", "act", "pool")
    NWIN = 8

    def __init__(self, nc):
        self.nc = nc
        self.ops = {e: [] for e in ("pe", "act", "dve", "pool", "sp")}
        self.res = {}
        self.ndma = {q: 0 for q in self.QUEUES}
        self.dma_ops = {q: [] for q in self.QUEUES}
        self.same_engine_sync = True
        self.phase = 0

    def _states(self, tok, create):
        name, sub = tok if isinstance(tok, tuple) else (tok, None)
        d = self.res.setdefault(name, {})
        if sub is None:
            if None not in d:
                d[None] = [None, []]
            return list(d.values())
        out = []
        if sub not in d:
            d[sub] = [None, []]
            if None in d:
                d[sub][0] = d[None][0]
                d[sub][1] = list(d[None][1])
        out.append(d[sub])
        return out

    def add(self, eng, fn, reads=(), writes=(), dma=False):
        op = _Op(eng, len(self.ops[eng]), fn, dma)
        op.phase = self.phase
        deps = set()
        for tok in reads:
            for st in self._states(tok, True):
                if st[0] is not None:
                    deps.add(st[0])
        for tok in writes:
            for st in self._states(tok, True):
                if st[0] is not None:
                    deps.add(st[0])
                for r in st[1]:
                    deps.add(r)
        for tok in reads:
            name, sub = tok if isinstance(tok, tuple) else (tok, None)
            d = self.res[name]
            if sub is None:
                for st in d.values():
                    st[1].append(op)
            else:
                d[sub][1].append(op)
        for tok in writes:
            name, sub = tok if isinstance(tok, tuple) else (tok, None)
            d = self.res[name]
            if sub is None:
                for k in list(d.keys()):
                    d[k] = [op, []]
            else:
                d[sub] = [op, []]
                if None in d:
                    pass
        deps.discard(op)
        op.deps = self._dedup(deps)
        self.ops[eng].append(op)
        if dma:
            q = eng
            op.pre = None
            n = self.ndma[q]
            self.ndma[q] += 1
            self.dma_ops[q].append(op)
            op.sem = (q, n % self.NWIN)
            op.val = 16 * (n // self.NWIN + 1)
            if n >= self.NWIN:
                op.pre = self.dma_ops[q][n - self.NWIN]
        return op

    @staticmethod
    def _dedup(deps):
        best = {}
        out = []
        for d in deps:
            if d.is_dma:
                out.append(d)
            else:
                b = best.get(d.eng)
                if b is None or d.idx > b.idx:
                    best[d.eng] = d
        out.extend(best.values())
        return out

    def emit(self):
        nc = self.nc
        for e, lst in self.ops.items():
            for op in lst:
                for d in op.deps:
                    if d.is_dma:
                        continue
                    if d.eng == op.eng and not op.is_dma:
                        if d.eng == "pe" or not self.same_engine_sync:
                            continue
                    d.signal = True
        for e in self.COMPUTE:
            c = 0
            ph = -1
            for op in self.ops[e]:
                if op.is_dma:
                    continue
                if op.phase != ph:
                    ph = op.phase
                    c = 0
                if op.signal:
                    c += 1
                    op.val = c
                    assert c < 30000, (e, ph, c)
        for q in self.QUEUES:
            for op in self.dma_ops[q]:
                assert op.val < 30000, (q, op.val)
        import contextlib
        with contextlib.ExitStack() as st:
            sems = {}
            for e in self.COMPUTE:
                for ph in sorted(set(op.phase for op in self.ops[e] if not op.is_dma)):
                    sems[(e, ph)] = st.enter_context(nc.semaphore("s_%s%d" % (e, ph)))
            for q in self.QUEUES:
                if not self.ndma[q]:
                    continue
                for i in range(self.NWIN):
                    sems[(q, i)] = st.enter_context(nc.semaphore("d_%s%d" % (q, i)))
            blk = st.enter_context(nc.Block())
            prog = self

            def run(ename, eobj):
                waited = {}
                waited_dma = set()
                for op in prog.ops[ename]:
                    waits = []
                    if op.is_dma and op.pre is not None:
                        waits.append(op.pre)
                    waits.extend(op.deps)
                    for d in waits:
                        if d.is_dma:
                            if id(d) in waited_dma:
                                continue
                            waited_dma.add(id(d))
                            eobj.wait_ge(sems[d.sem], d.val)
                        else:
                            if d.eng == ename and not op.is_dma:
                                if ename == "pe" or not prog.same_engine_sync:
                                    continue
                            if waited.get((d.eng, d.phase), 0) >= d.val:
                                continue
                            waited[(d.eng, d.phase)] = d.val
                            eobj.wait_ge(sems[(d.eng, d.phase)], d.val)
                    ins = op.fn(eobj)
                    if op.is_dma:
                        ins.then_inc(sems[op.sem], 16)
                    elif op.signal:
                        ins.then_inc(sems[(ename, op.phase)], 1)
                if ename in prog.QUEUES:
                    n = prog.ndma[ename]
                    for i in range(min(n, prog.NWIN)):
                        last = None
                        for op in reversed(prog.dma_ops[ename]):
                            if op.sem == (ename, i):
                                last = op
                                break
                        if last is not None:
                            eobj.wait_ge(sems[last.sem], last.val)

            @blk.tensor
            def _(e):
                run("pe", e)

            @blk.scalar
            def _(e):
                run("act", e)

            @blk.vector
            def _(e):
                run("dve", e)

            @blk.gpsimd
            def _(e):
                run("pool", e)

            @blk.sync
            def _(e):
                run("sp", e)

    def barrier(self):
        lasts = []
        for e in self.COMPUTE:
            for op in reversed(self.ops[e]):
                if not op.is_dma:
                    lasts.append(op)
                    break
        for q in self.QUEUES:
            seen = set()
            for op in reversed(self.dma_ops[q]):
                if op.sem not in seen:
                    seen.add(op.sem)
                    lasts.append(op)
                if len(seen) == self.NWIN:
                    break
        self.res = {}
        self._bar = lasts
        self.phase += 1

    def add_b(self, eng, fn, reads=(), writes=(), dma=False):
        op = self.add(eng, fn, reads, writes, dma)
        bar = getattr(self, "_bar", None)
        if bar:
            key = "_bdone_" + eng
            if getattr(self, key, None) is not bar:
                setattr(self, key, bar)
                s = set(op.deps)
                for b in bar:
                    if b is not op:
                        s.add(b)
                op.deps = self._dedup(s)
        return op


def _cdiv(a, b):
    return (a + b - 1) // b


import contextlib


class KB:
    def __init__(self, nc, NT, layers, final=True):
        self.nc = nc
        self.NT = NT
        self.NX = NT - N_META
        self.layers = layers
        self.final = final
        self.p = Prog(nc)
        self.tiles = [(t0, min(128, NT - t0)) for t0 in range(0, NT, 128)]
        self.ntile = len(self.tiles)
        self.nfull = NT // 128
        self.wslot = 0

    def op(self, eng, fn, reads=(), writes=()):
        return self.p.add_b(eng, fn, reads, writes, False)

    def dma(self, q, out, in_, reads=(), writes=()):
        return self.p.add_b(q, lambda e: e.dma_start(out=out, in_=in_), reads, writes, True)

    SHAPES = {
        "meta_tokens": (N_META, D),
        "ml_norm": (2, D), "ml_w_in": (2, D, ML_IN), "ml_b_i": (2, 8), "ml_b_f": (2, 8),
        "ml_h_gain": (2, D), "ml_w_out": (2, D, D),
        "ffn_norm": (2, D), "ffn_w_gate": (2, D, D_FF), "ffn_w_up": (2, D, D_FF), "ffn_w_down": (2, D_FF, D),
        "fox_norm": (2, D), "fox_w_in": (2, D, FOX_IN), "fox_b_f": (2, 32),
        "fox_q_gain": (2, 64), "fox_k_gain": (2, 64), "fox_w_out": (2, D, D),
        "moe_norm": (2, D), "moe_router": (2, D, 8),
        "moe_w_gate": (2, 8, D, D_FF), "moe_w_up": (2, 8, D, D_FF), "moe_w_down": (2, 8, D_FF, D),
        "final_norm": (1, D), "consts": (128, 5 * 128),
    }

    def declare(self):
        nc = self.nc
        NT, NX = self.NT, self.NX
        kb = self

        class Lazy(dict):
            def __missing__(self, name):
                shape = (NX, D) if name == "x" else kb.SHAPES[name]
                ap = nc.dram_tensor(name, list(shape), F32, kind="ExternalInput").ap()
                self[name] = ap
                kb.used_inputs.append(name)
                return ap

        self.used_inputs = []
        t = Lazy()
        t["out"] = nc.dram_tensor("out", [NX, D], F32, kind="ExternalOutput").ap()
        t["H"] = nc.dram_tensor("H", [NT, D], F32).ap()
        t["S_QT"] = nc.dram_tensor("S_QT", [16, 128, NT], BF16).ap()
        t["S_KT"] = nc.dram_tensor("S_KT", [16, 128, NT], BF16).ap()
        t["S_K"] = nc.dram_tensor("S_K", [NT, 1024], BF16).ap()
        t["S_V"] = nc.dram_tensor("S_V", [NT, D], BF16).ap()
        t["S_O"] = nc.dram_tensor("S_O", [NT, D], BF16).ap()
        t["S_Y"] = nc.dram_tensor("S_Y", [NT, D], BF16).ap()
        t["S_G"] = nc.dram_tensor("S_G", [NT, 32], F32).ap()
        self.t = t

    def sb(self, st, name, shape, dt):
        self._uid = getattr(self, "_uid", 0) + 1
        tl = st.enter_context(self.nc.sbuf_tensor("%s_%d" % (name, self._uid), list(shape), dt))
        self._ln = getattr(self, "_ln", {})
        self._ln[tl.name] = name
        return tl

    def ln(self, tl):
        return self._ln[tl.name]

    def build(self):
        nc = self.nc
        self.declare()
        t = self.t
        with contextlib.ExitStack() as st:
            self.pst = [st.enter_context(nc.psum_tensor("pt%d" % i, [128, 1024], BF16)) for i in range(2)]
            self.psa = [st.enter_context(nc.psum_tensor("pa%d" % i, [128, 512], F32)) for i in range(6)]
            cf = self.sb(st, "cf", [128, 640], F32)
            self.cf = cf
            self.ident_bf = self.sb(st, "ident_bf", [128, 128], BF16)
            self.mask_bf = self.sb(st, "mask_bf", [128, 128], BF16)
            self.dma("sp", cf[:, :], t["consts"][:, :], writes=["cf"])
            self.op("dve", lambda e: e.tensor_copy(out=self.ident_bf[:, :], in_=cf[:, 0:128]), reads=["cf"], writes=["ident_bf"])
            self.op("dve", lambda e: e.tensor_copy(out=self.mask_bf[:, :], in_=cf[:, 128:256]), reads=["cf"], writes=["mask_bf"])
            self.tri_f = cf[:, 128:256]
            self.ones_f = cf[:, 256:384]
            self.sel64 = cf[:, 384:512]
            self.sel8 = cf[:, 512:640]
            self.dma("sp", t["H"][0:N_META, :], t["meta_tokens"][:, :])
            self.dma("sp", t["H"][N_META:self.NT, :], t["x"][:, :])
            self.p.barrier()
            for kind, j in self.layers:
                getattr(self, "L_" + kind)(j)
                self.p.barrier()
            if self.final:
                self.L_final()
            self.p.emit()

    def bcast_row(self, st, name, src_row_ap, n, q="sp"):
        tl = self.sb(st, name, [128, n], F32)
        self.dma(q, tl[:, :], src_row_ap.partition_broadcast(128), writes=[name])
        return tl

    def rstd(self, ssum, tmp, out, inv_n, toks):
        self.op("dve", lambda e: e.tensor_scalar(out=tmp, in0=ssum, scalar1=inv_n, scalar2=EPS, op0=ALU.mult, op1=ALU.add), reads=toks, writes=toks)
        self.op("act", lambda e: e.activation(out=tmp, in_=tmp, func=AF.Sqrt), reads=toks, writes=toks)
        self.op("dve", lambda e: e.reciprocal(out=out, in_=tmp), reads=toks, writes=toks)

    def load_norm_T(self, bufs, i, gamma, xT, col0, it):
        t0, r = self.tiles[i]
        s = it % 2
        hs, xn, ss, junk = bufs["hs"], bufs["xn"], bufs["ss"], bufs["junk"]
        H = self.t["H"]
        self.dma("sp", hs[:r, s, :], H[t0:t0 + r, :], reads=[("H", i)], writes=[("hs", s)])
        self.op("act", lambda e: e.activation(out=junk[:r, :], in_=hs[:r, s, :], func=AF.Square, accum_out=ss[:r, s, 0:1]),
                reads=[("hs", s)], writes=["junk", ("ss", s)])
        self.rstd(ss[:r, s, 0:1], ss[:r, s, 1:2], ss[:r, s, 2:3], 1.0 / D, [("ss", s)])
        self.op("dve", lambda e: e.scalar_tensor_tensor(out=xn[:r, s, :], in0=hs[:r, s, :], scalar=ss[:r, s, 2:3], in1=gamma[:r, :], op0=ALU.mult, op1=ALU.mult),
                reads=[("hs", s), ("ss", s), self.ln(gamma)], writes=[("xn", s)])
        self.transpose_into(xn, s, r, xT, col0, "xn")
        return s

    def transpose_into(self, src, s, r, xT, col0, srcname):
        for half in range(2):
            pt = self.pst[half]
            for k in range(8):
                kc = half * 8 + k
                self.op("pe", lambda e, kc=kc, k=k, pt=pt: e.transpose(out=pt[:, k * 128:k * 128 + r], in_=src[:r, s, kc * 128:(kc + 1) * 128], identity=self.ident_bf[:r, :r]),
                        reads=[(srcname, s), "ident_bf"], writes=[("pt", half)])
            eng = "act" if half == 0 else "dve"
            src_ap = pt[:, :].rearrange("p (k c) -> p k c", k=8)[:, :, 0:r]
            dst_ap = xT[:, half * 8:(half + 1) * 8, col0:col0 + r]
            if eng == "act":
                self.op("act", lambda e, a=dst_ap, b=src_ap: e.copy(out=a, in_=b), reads=[("pt", half)], writes=[(self.ln(xT), col0 // 128)])
            else:
                self.op("dve", lambda e, a=dst_ap, b=src_ap: e.tensor_copy(out=a, in_=b), reads=[("pt", half)], writes=[(self.ln(xT), col0 // 128)])

    def wload(self, wt, nslots, src3, ncols, name):
        s = self.wslot_of.setdefault(wt.name, 0)
        self.wslot_of[wt.name] = (s + 1) % nslots
        kcn = src3.shape[1]
        self.dma("pool", wt[:, s, 0:kcn, 0:ncols], src3, writes=[(name, s)])
        return s

    wslot_of = {}

    GF = 5

    def L_ffn(self, j):
        t = self.t
        self._ffn(t["ffn_norm"][j:j + 1, :], [(t["ffn_w_gate"][j], t["ffn_w_up"][j], t["ffn_w_down"][j])], None)

    def L_moe(self, j):
        t = self.t
        ws = [(t["moe_w_gate"][j, e], t["moe_w_up"][j, e], t["moe_w_down"][j, e]) for e in range(N_EXP)]
        self._ffn(t["moe_norm"][j:j + 1, :], ws, t["moe_router"][j])

    def _ffn(self, gamma_row, weights, router):
        nc = self.nc
        H = self.t["H"]
        GF = self.GF
        GT = GF * 128
        with contextlib.ExitStack() as st:
            gam = self.bcast_row(st, "gam", gamma_row, D)
            bufs = {"hs": self.sb(st, "hs", [128, 2, D], F32), "xn": self.sb(st, "xn", [128, 2, D], BF16),
                    "ss": self.sb(st, "ss", [128, 2, 4], F32), "junk": self.sb(st, "junk", [128, D], BF16)}
            xT = self.sb(st, "xT", [128, KC, GT], BF16)
            aT = self.sb(st, "aT", [128, FC, GT], BF16)
            wg = self.sb(st, "wg", [128, 2, KC, 256], BF16)
            wu = self.sb(st, "wu", [128, 2, KC, 256], BF16)
            wd = self.sb(st, "wd", [128, 2, 11, 512], BF16)
            sg = self.sb(st, "sg", [128, 2, 512], F32)
            hr = self.sb(st, "hr", [128, 2, 512], F32)
            ho = self.sb(st, "ho", [128, 2, 512], F32)
            if router is not None:
                rw = self.sb(st, "rw", [128, KC, 8], BF16)
                self.dma("pool", rw[:, :, :], router.rearrange("(kc p) e -> p kc e", p=128), writes=["rw"])
                comb = self.sb(st, "comb", [128, GF, 8], F32)
                rt = self.sb(st, "rt", [128, GF, 40], F32)
            it = 0
            epi = 0
            for g0 in range(0, self.ntile, GF):
                gt = list(range(g0, min(g0 + GF, self.ntile)))
                ntok = sum(self.tiles[i][1] for i in gt)
                for li, i in enumerate(gt):
                    self.load_norm_T(bufs, i, gam, xT, li * 128, it)
                    it += 1
                if router is not None:
                    for li, i in enumerate(gt):
                        r = self.tiles[i][1]
                        ps = self.psa[li % 2]
                        for kc in range(KC):
                            self.op("pe", lambda e, kc=kc, li=li, r=r, ps=ps: e.matmul(ps[:r, 0:8], lhsT=xT[:, kc, li * 128:li * 128 + r], rhs=rw[:, kc, :], start=(kc == 0), stop=(kc == KC - 1)),
                                    reads=[("xT", li), "rw"], writes=[("pa", li % 2)])
                        L = rt[:r, li, 0:8]; m1 = rt[:r, li, 8:9]; eq = rt[:r, li, 16:24]; l2 = rt[:r, li, 24:32]; m2 = rt[:r, li, 9:10]
                        ex = rt[:r, li, 32:40]; dn = rt[:r, li, 10:11]; ge = rt[:r, li, 16:24]
                        tk = [("rt", li)]
                        self.op("dve", lambda e, L=L, ps=ps, r=r: e.tensor_copy(out=L, in_=ps[:r, 0:8]), reads=[("pa", li % 2)], writes=tk)
                        self.op("dve", lambda e, L=L, m1=m1: e.tensor_reduce(out=m1, in_=L, axis=AX.X, op=ALU.max), reads=tk, writes=tk)
                        self.op("dve", lambda e, L=L, m1=m1, eq=eq: e.tensor_scalar(out=eq, in0=L, scalar1=m1, scalar2=-1e30, op0=ALU.is_ge, op1=ALU.mult), reads=tk, writes=tk)
                        self.op("dve", lambda e, L=L, eq=eq, l2=l2: e.tensor_tensor(out=l2, in0=L, in1=eq, op=ALU.add), reads=tk, writes=tk)
                        self.op("dve", lambda e, l2=l2, m2=m2: e.tensor_reduce(out=m2, in_=l2, axis=AX.X, op=ALU.max), reads=tk, writes=tk)
                        self.op("dve", lambda e, L=L, m1=m1, l2=l2: e.tensor_scalar(out=l2, in0=L, scalar1=m1, scalar2=None, op0=ALU.subtract), reads=tk, writes=tk)
                        self.op("act", lambda e, l2=l2, ex=ex: e.activation(out=ex, in_=l2, func=AF.Exp), reads=tk, writes=tk)
                        self.op("dve", lambda e, L=L, m2=m2, ge=ge: e.tensor_scalar(out=ge, in0=L, scalar1=m2, scalar2=None, op0=ALU.is_ge), reads=tk, writes=tk)
                        self.op("dve", lambda e, ex=ex, ge=ge: e.tensor_tensor(out=ex, in0=ex, in1=ge, op=ALU.mult), reads=tk, writes=tk)
                        self.op("dve", lambda e, ex=ex, dn=dn: e.tensor_reduce(out=dn, in_=ex, axis=AX.X, op=ALU.add), reads=tk, writes=tk)
                        self.op("dve", lambda e, dn=dn: e.reciprocal(out=dn, in_=dn), reads=tk, writes=tk)
                        self.op("dve", lambda e, ex=ex, dn=dn, li=li, r=r: e.tensor_scalar(out=comb[:r, li, :], in0=ex, scalar1=dn, scalar2=None, op0=ALU.mult), reads=tk, writes=[("comb", li)])
                ctl = [(c0, min(512, ntok - c0)) for c0 in range(0, ntok, 512)]
                for ei, (Wg, Wu, Wd) in enumerate(weights):
                    Wg3 = Wg.rearrange("(kc p) f -> p kc f", p=128)
                    Wu3 = Wu.rearrange("(kc p) f -> p kc f", p=128)
                    Wd3 = Wd.rearrange("(fc p) d -> p fc d", p=128)
                    step = 0
                    for fg in range(FC // 2):
                        sgs = self.wload(wg, 2, Wg3[:, :, fg * 256:(fg + 1) * 256], 256, "wg")
                        sus = self.wload(wu, 2, Wu3[:, :, fg * 256:(fg + 1) * 256], 256, "wu")
                        for (c0, N) in ctl:
                            for fc in range(2):
                                b = (step % 2) * 2
                                step += 1
                                pg, pu = self.psa[b], self.psa[b + 1]
                                ctoks = [("xT", c) for c in range(c0 // 128, _cdiv(c0 + N, 128))]
                                for kc in range(KC):
                                    self.op("pe", lambda e, kc=kc, pg=pg, fc=fc, c0=c0, N=N, sgs=sgs: e.matmul(pg[:, 0:N], lhsT=wg[:, sgs, kc, fc * 128:(fc + 1) * 128], rhs=xT[:, kc, c0:c0 + N], start=(kc == 0), stop=(kc == KC - 1)),
                                            reads=[("wg", sgs)] + ctoks, writes=[("pa", b)])
                                for kc in range(KC):
                                    self.op("pe", lambda e, kc=kc, pu=pu, fc=fc, c0=c0, N=N, sus=sus: e.matmul(pu[:, 0:N], lhsT=wu[:, sus, kc, fc * 128:(fc + 1) * 128], rhs=xT[:, kc, c0:c0 + N], start=(kc == 0), stop=(kc == KC - 1)),
                                            reads=[("wu", sus)] + ctoks, writes=[("pa", b + 1)])
                                ss_ = step % 2
                                self.op("act", lambda e, pg=pg, N=N, ss_=ss_: e.activation(out=sg[:, ss_, 0:N], in_=pg[:, 0:N], func=AF.Silu),
                                        reads=[("pa", b)], writes=[("sg", ss_)])
                                fch = fg * 2 + fc
                                self.op("dve", lambda e, pu=pu, N=N, ss_=ss_, fch=fch, c0=c0: e.tensor_tensor(out=aT[:, fch, c0:c0 + N], in0=sg[:, ss_, 0:N], in1=pu[:, 0:N], op=ALU.mult),
                                        reads=[("sg", ss_), ("pa", b + 1)], writes=[("aT", fch)])
                    for dt in range(4):
                        for q4 in range(4):
                            sd = self.wload(wd, 2, Wd3[:, q4 * 11:(q4 + 1) * 11, dt * 512:(dt + 1) * 512], 512, "wd")
                            for jj in range(11):
                                fch = q4 * 11 + jj
                                for li, i in enumerate(gt):
                                    r = self.tiles[i][1]
                                    self.op("pe", lambda e, li=li, r=r, fch=fch, jj=jj, sd=sd: e.matmul(self.psa[li][:r, :], lhsT=aT[:, fch, li * 128:li * 128 + r], rhs=wd[:, sd, jj, :], start=(fch == 0), stop=(fch == FC - 1)),
                                            reads=[("aT", fch), ("wd", sd)], writes=[("pa", li)])
                        for li, i in enumerate(gt):
                            t0, r = self.tiles[i]
                            s = epi % 2
                            epi += 1
                            self.dma("sp", hr[:r, s, :], H[t0:t0 + r, dt * 512:(dt + 1) * 512], reads=[("H", i)], writes=[("hr", s)])
                            if router is None:
                                self.op("dve", lambda e, li=li, r=r, s=s: e.tensor_tensor(out=ho[:r, s, :], in0=self.psa[li][:r, :], in1=hr[:r, s, :], op=ALU.add),
                                        reads=[("pa", li), ("hr", s)], writes=[("ho", s)])
                            else:
                                self.op("dve", lambda e, li=li, r=r, s=s, ei=ei: e.scalar_tensor_tensor(out=ho[:r, s, :], in0=self.psa[li][:r, :], scalar=comb[:r, li, ei:ei + 1], in1=hr[:r, s, :], op0=ALU.mult, op1=ALU.add),
                                        reads=[("pa", li), ("hr", s), ("comb", li)], writes=[("ho", s)])
                            self.dma("sp", H[t0:t0 + r, dt * 512:(dt + 1) * 512], ho[:r, s, :], reads=[("ho", s)], writes=[("H", i)])

    GP = 4

    def groups(self):
        for g0 in range(0, self.ntile, self.GP):
            gt = list(range(g0, min(g0 + self.GP, self.ntile)))
            ntok = sum(self.tiles[i][1] for i in gt)
            yield gt, self.tiles[g0][0], ntok

    def a_style(self, xT, w, ws, li, r, ps, ncols, wname):
        for kc in range(KC):
            self.op("pe", lambda e, kc=kc: e.matmul(ps[:r, 0:ncols], lhsT=xT[:, kc, li * 128:li * 128 + r], rhs=w[:, ws, kc, 0:ncols], start=(kc == 0), stop=(kc == KC - 1)),
                    reads=[(self.ln(xT), li), (wname, ws)], writes=[("pa", self._psidx(ps))])

    def b_style(self, xT, w, ws, c0, ps, ntok, gt, wname):
        toks = [(self.ln(xT), li) for li in range(len(gt))]
        for kc in range(KC):
            self.op("pe", lambda e, kc=kc: e.matmul(ps[:, 0:ntok], lhsT=w[:, ws, kc, c0:c0 + 128], rhs=xT[:, kc, 0:ntok], start=(kc == 0), stop=(kc == KC - 1)),
                    reads=toks + [(wname, ws)], writes=[("pa", self._psidx(ps))])

    def _psidx(self, ps):
        for i, p in enumerate(self.psa):
            if p is ps:
                return i
        raise KeyError

    def evac(self, k, out, in_, reads, writes, func=None, scale=1.0):
        if func is not None or k % 2 == 0:
            f = func if func is not None else AF.Copy
            self.op("act", lambda e: e.activation(out=out, in_=in_, func=f, scale=scale), reads=reads, writes=writes)
        else:
            if scale == 1.0:
                self.op("dve", lambda e: e.tensor_copy(out=out, in_=in_), reads=reads, writes=writes)
            else:
                self.op("dve", lambda e: e.tensor_scalar(out=out, in0=in_, scalar1=scale, scalar2=None, op0=ALU.mult), reads=reads, writes=writes)

    def out_proj(self, W):
        H, SY = self.t["H"], self.t["S_Y"]
        W3 = W.rearrange("(kc p) f -> p kc f", p=128)
        with contextlib.ExitStack() as st:
            yt = self.sb(st, "yt", [128, 2, D], BF16)
            xT = self.sb(st, "xT", [128, KC, self.GP * 128], BF16)
            w = self.sb(st, "w", [128, 2, KC, 512], BF16)
            hr = self.sb(st, "hr", [128, 2, 512], F32)
            ho = self.sb(st, "ho", [128, 2, 512], F32)
            it = 0
            epi = 0
            for gt, tok0, ntok in self.groups():
                for li, i in enumerate(gt):
                    t0, r = self.tiles[i]
                    s = it % 2
                    it += 1
                    self.dma("sp", yt[:r, s, :], SY[t0:t0 + r, :], writes=[("yt", s)])
                    self.transpose_into(yt, s, r, xT, li * 128, "yt")
                for dt in range(4):
                    ws = self.wload(w, 2, W3[:, :, dt * 512:(dt + 1) * 512], 512, "w")
                    for li, i in enumerate(gt):
                        t0, r = self.tiles[i]
                        ps = self.psa[(epi % 4)]
                        self.a_style(xT, w, ws, li, r, ps, 512, "w")
                        s = epi % 2
                        epi += 1
                        self.dma("sp", hr[:r, s, :], H[t0:t0 + r, dt * 512:(dt + 1) * 512], reads=[("H", i)], writes=[("hr", s)])
                        self.op("dve", lambda e, r=r, s=s, ps=ps: e.tensor_tensor(out=ho[:r, s, :], in0=ps[:r, :], in1=hr[:r, s, :], op=ALU.add),
                                reads=[("pa", self._psidx(ps)), ("hr", s)], writes=[("ho", s)])
                        self.dma("sp", H[t0:t0 + r, dt * 512:(dt + 1) * 512], ho[:r, s, :], reads=[("ho", s)], writes=[("H", i)])

    def load_tiled(self, dst, slot, src, c0, ncols, name):
        nf = self.nfull
        if nf:
            self.dma("sp", dst[:, slot, 0:nf, 0:ncols], src[0:nf * 128, c0:c0 + ncols].rearrange("(i p) c -> p i c", p=128), writes=[(name, slot)])
        if self.ntile > nf:
            t0, r = self.tiles[nf]
            self.dma("sp", dst[:r, slot, nf, 0:ncols], src[t0:t0 + r, c0:c0 + ncols], writes=[(name, slot)])

    def store_tiled(self, dst, c0, ncols, src, slot, name):
        nf = self.nfull
        if nf:
            self.dma("sp", dst[0:nf * 128, c0:c0 + ncols].rearrange("(i p) c -> p i c", p=128), src[:, slot, 0:nf, 0:ncols], reads=[(name, slot)])
        if self.ntile > nf:
            t0, r = self.tiles[nf]
            self.dma("sp", dst[t0:t0 + r, c0:c0 + ncols], src[:r, slot, nf, 0:ncols], reads=[(name, slot)])

    def L_ml(self, j):
        t = self.t
        NT, ntile = self.NT, self.ntile
        Win = t["ml_w_in"][j].rearrange("(kc p) f -> p kc f", p=128)
        SQT, SKT, SK, SV, SO, SG, SY = t["S_QT"], t["S_KT"], t["S_K"], t["S_V"], t["S_O"], t["S_G"], t["S_Y"]
        with contextlib.ExitStack() as st:
            gam = self.bcast_row(st, "gam", t["ml_norm"][j:j + 1, :], D)
            bufs = {"hs": self.sb(st, "hs", [128, 2, D], F32), "xn": self.sb(st, "xn", [128, 2, D], BF16),
                    "ss": self.sb(st, "ss", [128, 2, 4], F32), "junk": self.sb(st, "junk", [128, D], BF16)}
            xT = self.sb(st, "xT", [128, KC, self.GP * 128], BF16)
            w = self.sb(st, "w", [128, 3, KC, 512], BF16)
            sg = self.sb(st, "stg", [128, 4, 512], BF16)
            sgf = self.sb(st, "stgf", [128, 2, 16], F32)
            it = 0
            ev = 0
            for gt, tok0, ntok in self.groups():
                for li, i in enumerate(gt):
                    self.load_norm_T(bufs, i, gam, xT, li * 128, it)
                    it += 1
                for sec, ntl in (("q", 2), ("k", 2), ("v", 4), ("o", 4), ("g", 1)):
                    base = {"q": 0, "k": 1024, "v": 2048, "o": 4096, "g": 6144}[sec]
                    for wt in range(ntl):
                        ncols = 16 if sec == "g" else 512
                        ws = self.wload(w, 3, Win[:, :, base + wt * 512:base + wt * 512 + ncols], ncols, "w")
                        if sec in ("q", "k"):
                            dst = SQT if sec == "q" else SKT
                            scl = 1.0 if sec == "q" else ML_DQK ** -0.5
                            for hh in range(4):
                                h = wt * 4 + hh
                                ps = self.psa[ev % 4]
                                self.b_style(xT, w, ws, hh * 128, ps, ntok, gt, "w")
                                s = ev % 4
                                self.evac(ev, sg[:, s, 0:ntok], ps[:, 0:ntok], [("pa", ev % 4)], [("stg", s)], scale=scl)
                                ev += 1
                                self.dma("sp", dst[h, :, tok0:tok0 + ntok], sg[:, s, 0:ntok], reads=[("stg", s)])
                        if sec in ("k", "v", "o", "g"):
                            for li, i in enumerate(gt):
                                t0, r = self.tiles[i]
                                ps = self.psa[ev % 4]
                                self.a_style(xT, w, ws, li, r, ps, ncols, "w")
                                s = ev % 4
                                if sec == "g":
                                    s2 = ev % 2
                                    self.evac(1, sgf[:r, s2, :], ps[:r, 0:16], [("pa", ev % 4)], [("stgf", s2)])
                                    self.dma("sp", SG[t0:t0 + r, 0:16], sgf[:r, s2, :], reads=[("stgf", s2)])
                                else:
                                    if sec == "k":
                                        self.evac(ev, sg[:r, s, :], ps[:r, :], [("pa", ev % 4)], [("stg", s)], scale=ML_DQK ** -0.5)
                                        d2 = SK[t0:t0 + r, wt * 512:(wt + 1) * 512]
                                    elif sec == "v":
                                        self.evac(ev, sg[:r, s, :], ps[:r, :], [("pa", ev % 4)], [("stg", s)])
                                        d2 = SV[t0:t0 + r, wt * 512:(wt + 1) * 512]
                                    else:
                                        self.evac(ev, sg[:r, s, :], ps[:r, :], [("pa", ev % 4)], [("stg", s)], func=AF.Sigmoid)
                                        d2 = SO[t0:t0 + r, wt * 512:(wt + 1) * 512]
                                    self.dma("sp", d2, sg[:r, s, :], reads=[("stg", s)])
                                ev += 1
        self.p.barrier()
        with contextlib.ExitStack() as st:
            G = self.sb(st, "G", [128, ntile, 16], F32)
            bi = self.bcast_row(st, "bi", t["ml_b_i"][j:j + 1, :], 8)
            bf = self.bcast_row(st, "bf", t["ml_b_f"][j:j + 1, :], 8)
            hg = self.bcast_row(st, "hg", t["ml_h_gain"][j:j + 1, :], D)
            TI = self.sb(st, "TI", [128, ntile, 8], F32)
            TF = self.sb(st, "TF", [128, ntile, 8], F32)
            WS = self.sb(st, "WS", [128, ntile, 8], F32)
            ET = self.sb(st, "ET", [128, ntile, 8], F32)
            EG = self.sb(st, "EG", [128, ntile, 8], F32)
            self.op("dve", lambda e: e.memset(G[:, :, :], 0.0), writes=["G"])
            for i, (t0, r) in enumerate(self.tiles):
                self.dma("sp", G[:r, i, :], SG[t0:t0 + r, 0:16], writes=["G"])
            self.op("dve", lambda e: e.tensor_tensor(out=TI[:, :, :], in0=G[:, :, 0:8], in1=bi[:, 0:8].unsqueeze(1).broadcast_to([128, ntile, 8]), op=ALU.add), reads=["G", "bi"], writes=["TI"])
            self.op("dve", lambda e: e.tensor_tensor(out=TF[:, :, :], in0=G[:, :, 8:16], in1=bf[:, 0:8].unsqueeze(1).broadcast_to([128, ntile, 8]), op=ALU.add), reads=["G", "bf"], writes=["TF"])
            self.op("act", lambda e: e.activation(out=TI[:, :, :], in_=TI[:, :, :], func=AF.Tanh, scale=1.0 / GATE_CAP), reads=["TI"], writes=["TI"])
            self.op("act", lambda e: e.activation(out=TF[:, :, :], in_=TF[:, :, :], func=AF.Tanh, scale=1.0 / GATE_CAP), reads=["TF"], writes=["TF"])
            self.op("act", lambda e: e.activation(out=TF[:, :, :], in_=TF[:, :, :], func=AF.Exp, scale=-GATE_CAP), reads=["TF"], writes=["TF"])
            self.op("act", lambda e: e.activation(out=TF[:, :, :], in_=TF[:, :, :], func=AF.Ln, bias=1.0), reads=["TF"], writes=["TF"])
            pnb, png = self.psa[0], self.psa[1]
            for i, (t0, r) in enumerate(self.tiles):
                self.op("pe", lambda e, i=i, r=r: e.matmul(pnb[:r, i * 8:(i + 1) * 8], lhsT=self.tri_f[:r, :r], rhs=TF[:r, i, :], start=True, stop=True), reads=["TF", "cf"], writes=[("pa", 0)])
                self.op("pe", lambda e, i=i, r=r: e.matmul(png[:, i * 8:(i + 1) * 8], lhsT=self.ones_f[:r, :], rhs=TF[:r, i, :], start=True, stop=True), reads=["TF", "cf"], writes=[("pa", 1)])
            nb3 = pnb[:, 0:ntile * 8].rearrange("p (i h) -> p i h", h=8)
            ng3 = png[:, 0:ntile * 8].rearrange("p (i h) -> p i h", h=8)
            self.op("dve", lambda e: e.scalar_tensor_tensor(out=WS[:, :, :], in0=TI[:, :, :], scalar=GATE_CAP, in1=nb3, op0=ALU.mult, op1=ALU.add), reads=["TI", ("pa", 0)], writes=["WS"])
            self.op("act", lambda e: e.activation(out=WS[:, :, :], in_=WS[:, :, :], func=AF.Exp), reads=["WS"], writes=["WS"])
            self.op("act", lambda e: e.activation(out=ET[:, :, :], in_=nb3, func=AF.Exp, scale=-1.0), reads=[("pa", 0)], writes=["ET"])
            self.op("act", lambda e: e.activation(out=EG[:, :, :], in_=ng3, func=AF.Exp, scale=-1.0), reads=[("pa", 1)], writes=["EG"])
            QT = self.sb(st, "QT", [128, 2, NT], BF16)
            KT = self.sb(st, "KT", [128, 2, NT], BF16)
            Kh = self.sb(st, "Kh", [128, 2, ntile, 128], BF16)
            Vh = self.sb(st, "Vh", [128, 2, ntile, 257], BF16)
            Oh = self.sb(st, "Oh", [128, 2, ntile, 256], BF16)
            Yh = self.sb(st, "Yh", [128, 2, ntile, 256], BF16)
            Z = self.sb(st, "Z", [128, 257], F32)
            Cb = self.sb(st, "Cb", [128, 2, 257], BF16)
            Sm = self.sb(st, "Sm", [128, 2, 128], BF16)
            Vp = self.sb(st, "Vp", [128, 2, 257], BF16)
            dd = self.sb(st, "dd", [128, 2, 8], F32)
            hh_ = self.sb(st, "hh", [128, 2, 256], F32)
            y1 = self.sb(st, "y1", [128, 2, 256], F32)
            jk = self.sb(st, "jk", [128, 256], BF16)
            self.op("dve", lambda e: e.memset(Vh[:, :, :, 256:257], 1.0), writes=["Vh"])
            cnt = 0
            for h in range(ML_H):
                hs_ = h % 2
                self.dma("sp", QT[:, hs_, :], SQT[h, :, :], writes=[("QT", hs_)])
                self.dma("sp", KT[:, hs_, :], SKT[h, :, :], writes=[("KT", hs_)])
                self.load_tiled(Kh, hs_, SK, h * 128, 128, "Kh")
                self.load_tiled(Vh, hs_, SV, h * 256, 256, "Vh")
                self.load_tiled(Oh, hs_, SO, h * 256, 256, "Oh")
                for i, (t0, r) in enumerate(self.tiles):
                    c = cnt % 2
                    cnt += 1
                    ps_s, ps_x, ps_c = self.psa[c], self.psa[2 + c], self.psa[4 + c]
                    self.op("pe", lambda e, hs_=hs_, t0=t0, r=r, ps_s=ps_s: e.matmul(ps_s[:r, 0:r], lhsT=KT[:, hs_, t0:t0 + r], rhs=QT[:, hs_, t0:t0 + r], start=True, stop=True),
                            reads=[("KT", hs_), ("QT", hs_)], writes=[("pa", c)])
                    self.op("dve", lambda e, r=r, c=c, ps_s=ps_s: e.tensor_tensor(out=Sm[:r, c, 0:r], in0=ps_s[:r, 0:r], in1=self.mask_bf[:r, 0:r], op=ALU.mult),
                            reads=[("pa", c), "mask_bf"], writes=[("Sm", c)])
                    self.op("act", lambda e, r=r, c=c, hs_=hs_, i=i, h=h: e.activation(out=Vp[:r, c, :], in_=Vh[:r, hs_, i, :], func=AF.Copy, scale=WS[:r, i, h:h + 1]),
                            reads=[("Vh", hs_), "WS"], writes=[("Vp", c)])
                    self.op("pe", lambda e, r=r, c=c, ps_x=ps_x, i=i: e.matmul(ps_x[:r, 0:257], lhsT=Sm[:r, c, 0:r], rhs=Vp[:r, c, :], start=True, stop=(i == 0)),
                            reads=[("Sm", c), ("Vp", c)], writes=[("pa", 2 + c)])
                    if i > 0:
                        cb = (i - 1) % 2
                        self.op("pe", lambda e, r=r, ps_x=ps_x, hs_=hs_, t0=t0, cb=cb: e.matmul(ps_x[:r, 0:257], lhsT=QT[:, hs_, t0:t0 + r], rhs=Cb[:, cb, :], start=False, stop=True),
                                reads=[("QT", hs_), ("Cb", cb)], writes=[("pa", 2 + c)])
                    if i < ntile - 1:
                        self.op("pe", lambda e, r=r, c=c, ps_c=ps_c, hs_=hs_, i=i: e.matmul(ps_c[:, 0:257], lhsT=Kh[:r, hs_, i, :], rhs=Vp[:r, c, :], start=True, stop=True),
                                reads=[("Kh", hs_), ("Vp", c)], writes=[("pa", 4 + c)])
                        if i == 0:
                            self.op("dve", lambda e, ps_c=ps_c: e.tensor_copy(out=Z[:, :], in_=ps_c[:, 0:257]), reads=[("pa", 4 + c)], writes=["Z"])
                        else:
                            self.op("dve", lambda e, ps_c=ps_c, i=i, h=h: e.scalar_tensor_tensor(out=Z[:, :], in0=Z[:, :], scalar=EG[:, i - 1, h:h + 1], in1=ps_c[:, 0:257], op0=ALU.mult, op1=ALU.add),
                                    reads=[("pa", 4 + c), "Z", "EG"], writes=["Z"])
                        self.op("act", lambda e, i=i, h=h: e.activation(out=Cb[:, i % 2, :], in_=Z[:, :], func=AF.Copy, scale=EG[:, i, h:h + 1]),
                                reads=["Z", "EG"], writes=[("Cb", i % 2)])
                    tk = [("dd", c)]
                    self.op("act", lambda e, r=r, c=c, ps_x=ps_x, i=i, h=h: e.activation(out=dd[:r, c, 0:1], in_=ps_x[:r, 256:257], func=AF.Abs, scale=ET[:r, i, h:h + 1]),
                            reads=[("pa", 2 + c), "ET"], writes=tk)
                    self.op("dve", lambda e, r=r, c=c: e.tensor_scalar(out=dd[:r, c, 1:2], in0=dd[:r, c, 0:1], scalar1=1.0, scalar2=None, op0=ALU.max), reads=tk, writes=tk)
                    self.op("dve", lambda e, r=r, c=c: e.reciprocal(out=dd[:r, c, 2:3], in_=dd[:r, c, 1:2]), reads=tk, writes=tk)
                    self.op("dve", lambda e, r=r, c=c, i=i, h=h: e.tensor_tensor(out=dd[:r, c, 3:4], in0=dd[:r, c, 2:3], in1=ET[:r, i, h:h + 1], op=ALU.mult), reads=tk + ["ET"], writes=tk)
                    self.op("act", lambda e, r=r, c=c, ps_x=ps_x: e.activation(out=hh_[:r, c, :], in_=ps_x[:r, 0:256], func=AF.Copy, scale=dd[:r, c, 3:4]),
                            reads=[("pa", 2 + c)] + tk, writes=[("hh", c)])
                    self.op("act", lambda e, r=r, c=c: e.activation(out=jk[:r, :], in_=hh_[:r, c, :], func=AF.Square, accum_out=dd[:r, c, 4:5]),
                            reads=[("hh", c)], writes=["jk"] + tk)
                    self.rstd(dd[:r, c, 4:5], dd[:r, c, 5:6], dd[:r, c, 6:7], 1.0 / ML_DV, tk)
                    self.op("dve", lambda e, r=r, c=c, h=h: e.scalar_tensor_tensor(out=y1[:r, c, :], in0=hh_[:r, c, :], scalar=dd[:r, c, 6:7], in1=hg[:r, h * 256:(h + 1) * 256], op0=ALU.mult, op1=ALU.mult),
                            reads=[("hh", c), "hg"] + tk, writes=[("y1", c)])
                    self.op("dve", lambda e, r=r, c=c, hs_=hs_, i=i: e.tensor_tensor(out=Yh[:r, hs_, i, :], in0=y1[:r, c, :], in1=Oh[:r, hs_, i, :], op=ALU.mult),
                            reads=[("y1", c), ("Oh", hs_)], writes=[("Yh", hs_)])
                self.store_tiled(SY, h * 256, 256, Yh, hs_, "Yh")
        self.p.barrier()
        self.out_proj(t["ml_w_out"][j])

    def L_fox(self, j):
        t = self.t
        NT, ntile = self.NT, self.ntile
        Win = t["fox_w_in"][j].rearrange("(kc p) f -> p kc f", p=128)
        SQT, SKT, SV, SO, SG, SY = t["S_QT"], t["S_KT"], t["S_V"], t["S_O"], t["S_G"], t["S_Y"]
        GT = self.GP * 128
        with contextlib.ExitStack() as st:
            gam = self.bcast_row(st, "gam", t["fox_norm"][j:j + 1, :], D)
            gq = self.bcast_row(st, "gq", t["fox_q_gain"][j:j + 1, :], 64)
            gk = self.bcast_row(st, "gk", t["fox_k_gain"][j:j + 1, :], 64)
            bfb = self.bcast_row(st, "bfb", t["fox_b_f"][j:j + 1, :], 32)
            self.op("dve", lambda e: e.tensor_scalar(out=gq[:, :], in0=gq[:, :], scalar1=FOX_DH ** -0.5, scalar2=None, op0=ALU.mult), reads=["gq"], writes=["gq"])
            bufs = {"hs": self.sb(st, "hs", [128, 2, D], F32), "xn": self.sb(st, "xn", [128, 2, D], BF16),
                    "ss": self.sb(st, "ss", [128, 2, 4], F32), "junk": self.sb(st, "junk", [128, D], BF16)}
            xT = self.sb(st, "xT", [128, KC, GT], BF16)
            w = self.sb(st, "w", [128, 3, KC, 512], BF16)
            sg = self.sb(st, "stg", [128, 4, 512], BF16)
            sq = self.sb(st, "sq", [128, 2, 512], F32)
            qn = self.sb(st, "qn", [128, 2, 512], F32)
            rs = self.sb(st, "rs", [128, 2, 24], F32)
            qrow = self.sb(st, "qrow", [128, 2 * self.GP, D], BF16)
            QTs = self.sb(st, "QTs", [128, KC, GT], BF16)
            lfs = self.sb(st, "lfs", [128, 2, 32], F32)
            it = 0
            ev = 0
            for gt, tok0, ntok in self.groups():
                for li, i in enumerate(gt):
                    self.load_norm_T(bufs, i, gam, xT, li * 128, it)
                    it += 1
                for sec, ntl in (("q", 4), ("k", 4), ("v", 4), ("o", 4), ("f", 1)):
                    base = {"q": 0, "k": 2048, "v": 4096, "o": 6144, "f": 8192}[sec]
                    for wt in range(ntl):
                        ncols = 32 if sec == "f" else 512
                        ws = self.wload(w, 3, Win[:, :, base + wt * 512:base + wt * 512 + ncols], ncols, "w")
                        for li, i in enumerate(gt):
                            t0, r = self.tiles[i]
                            pi = ev % 4
                            ps = self.psa[pi]
                            self.a_style(xT, w, ws, li, r, ps, ncols, "w")
                            s = ev % 4
                            s2 = ev % 2
                            if sec in ("q", "k"):
                                gn = gq if sec == "q" else gk
                                qi_ = li + (0 if sec == "q" else self.GP)
                                tk = [("rs", s2)]
                                self.op("act", lambda e, r=r, s2=s2, ps=ps: e.activation(out=sq[:r, s2, :], in_=ps[:r, :], func=AF.Square), reads=[("pa", pi)], writes=[("sq", s2)])
                                self.op("dve", lambda e, r=r, s2=s2: e.tensor_reduce(out=rs[:r, s2, 0:8], in_=sq[:r, s2, :].rearrange("p (h d) -> p h d", d=64), axis=AX.X, op=ALU.add), reads=[("sq", s2)], writes=tk)
                                self.rstd(rs[:r, s2, 0:8], rs[:r, s2, 8:16], rs[:r, s2, 16:24], 1.0 / FOX_DH, tk)
                                self.op("dve", lambda e, r=r, s2=s2, ps=ps: e.tensor_tensor(out=qn[:r, s2, :].rearrange("p (h d) -> p h d", d=64), in0=ps[:r, :].rearrange("p (h d) -> p h d", d=64),
                                                                                             in1=rs[:r, s2, 16:24].unsqueeze(2).broadcast_to([r, 8, 64]), op=ALU.mult),
                                        reads=[("pa", pi)] + tk, writes=[("qn", s2)])
                                self.op("dve", lambda e, r=r, s2=s2, gn=gn, qi_=qi_, wt=wt: e.tensor_tensor(out=qrow[:r, qi_, wt * 512:(wt + 1) * 512].rearrange("p (h d) -> p h d", d=64), in0=qn[:r, s2, :].rearrange("p (h d) -> p h d", d=64),
                                                                                                                 in1=gn[:r, 0:64].unsqueeze(1).broadcast_to([r, 8, 64]), op=ALU.mult),
                                        reads=[("qn", s2), self.ln(gn)], writes=[("qrow", qi_)])
                            elif sec == "v":
                                self.evac(ev, sg[:r, s, :], ps[:r, :], [("pa", pi)], [("stg", s)])
                                self.dma("sp", SV[t0:t0 + r, wt * 512:(wt + 1) * 512], sg[:r, s, :], reads=[("stg", s)])
                            elif sec == "o":
                                self.evac(ev, sg[:r, s, :], ps[:r, :], [("pa", pi)], [("stg", s)], func=AF.Sigmoid)
                                self.dma("sp", SO[t0:t0 + r, wt * 512:(wt + 1) * 512], sg[:r, s, :], reads=[("stg", s)])
                            else:
                                tk = [("lfs", s2)]
                                self.op("dve", lambda e, r=r, s2=s2, ps=ps: e.tensor_tensor(out=lfs[:r, s2, :], in0=ps[:r, 0:32], in1=bfb[:r, :], op=ALU.add), reads=[("pa", pi), "bfb"], writes=tk)
                                self.op("act", lambda e, r=r, s2=s2: e.activation(out=lfs[:r, s2, :], in_=lfs[:r, s2, :], func=AF.Exp, scale=-1.0), reads=tk, writes=tk)
                                self.op("act", lambda e, r=r, s2=s2: e.activation(out=lfs[:r, s2, :], in_=lfs[:r, s2, :], func=AF.Ln, bias=1.0), reads=tk, writes=tk)
                                self.dma("sp", SG[t0:t0 + r, 0:32], lfs[:r, s2, :], reads=tk)
                            ev += 1
                    if sec in ("q", "k"):
                        dst = SQT if sec == "q" else SKT
                        for li, i in enumerate(gt):
                            r = self.tiles[i][1]
                            qi_ = li + (0 if sec == "q" else self.GP)
                            self.transpose_into(qrow, qi_, r, QTs, li * 128, "qrow")
                        self.dma("sp", dst[:, :, tok0:tok0 + ntok].rearrange("k p t -> p k t"), QTs[:, :, 0:ntok], reads=["QTs"])
        self.p.barrier()
        with contextlib.ExitStack() as st:
            G = self.sb(st, "G", [128, ntile, 32], F32)
            NC = self.sb(st, "NC", [128, ntile, 32], F32)
            NR = self.sb(st, "NR", [128, ntile, 32], F32)
            carry = self.sb(st, "carry", [128, 32], F32)
            self.op("dve", lambda e: e.memset(G[:, :, :], 0.0), writes=["G"])
            self.op("dve", lambda e: e.memset(carry[:, :], 0.0), writes=["carry"])
            for i, (t0, r) in enumerate(self.tiles):
                self.dma("sp", G[:r, i, :], SG[t0:t0 + r, 0:32], writes=["G"])
            for i, (t0, r) in enumerate(self.tiles):
                p1, p2, p3 = self.psa[(i % 2) * 3], self.psa[(i % 2) * 3 + 1], self.psa[(i % 2) * 3 + 2]
                b = (i % 2) * 3
                self.op("pe", lambda e, i=i, r=r, p1=p1: e.matmul(p1[:r, 0:32], lhsT=self.tri_f[:r, :r], rhs=G[:r, i, :], start=True, stop=True), reads=["G", "cf"], writes=[("pa", b)])
                self.op("pe", lambda e, i=i, r=r, p2=p2: e.matmul(p2[:, 0:32], lhsT=self.ones_f[:r, :], rhs=G[:r, i, :], start=True, stop=True), reads=["G", "cf"], writes=[("pa", b + 1)])
                self.op("dve", lambda e, i=i, r=r, p1=p1: e.tensor_tensor(out=NC[:r, i, :], in0=p1[:r, 0:32], in1=carry[:r, :], op=ALU.add), reads=[("pa", b), "carry"], writes=[("NC", i)])
                self.op("dve", lambda e, p2=p2: e.tensor_tensor(out=carry[:, :], in0=p2[:, 0:32], in1=carry[:, :], op=ALU.add), reads=[("pa", b + 1), "carry"], writes=["carry"])
                sel = self.sel64 if r == 128 else self.sel8
                self.op("pe", lambda e, i=i, r=r, p3=p3, sel=sel: e.matmul(p3[:, 0:32], lhsT=sel[:r, :], rhs=NC[:r, i, :], start=True, stop=True), reads=[("NC", i), "cf"], writes=[("pa", b + 2)])
                self.op("act", lambda e, i=i, p3=p3: e.copy(out=NR[:, i, :], in_=p3[:, 0:32]), reads=[("pa", b + 2)], writes=[("NR", i)])
            QT = self.sb(st, "QT", [128, 2, NT], BF16)
            KT = self.sb(st, "KT", [128, 2, NT], BF16)
            Vp = self.sb(st, "Vp", [128, 2, ntile, 2, 65], BF16)
            Op = self.sb(st, "Op", [128, 2, ntile, 128], BF16)
            Yp = self.sb(st, "Yp", [128, 2, ntile, 128], BF16)
            bias = self.sb(st, "bias", [128, 2, ntile, 2], F32)
            P = self.sb(st, "P", [128, 4, 512], BF16)
            rd = self.sb(st, "rd", [128, 4, 2], F32)
            self.op("dve", lambda e: e.memset(Vp[:, :, :, :, 64:65], 1.0), writes=["Vp"])
            nf = self.nfull
            scnt = 0
            bcnt = 0
            for p in range(16):
                ps_ = p % 2
                self.dma("sp", QT[:, ps_, :], SQT[p, :, :], writes=[("QT", ps_)])
                self.dma("sp", KT[:, ps_, :], SKT[p, :, :], writes=[("KT", ps_)])
                if nf:
                    for hh in range(2):
                        self.dma("sp", Vp[:, ps_, 0:nf, hh, 0:64], SV[0:nf * 128, p * 128 + hh * 64:p * 128 + (hh + 1) * 64].rearrange("(i q) d -> q i d", q=128), writes=[("Vp", ps_)])
                if ntile > nf:
                    t0, r = self.tiles[nf]
                    self.dma("sp", Vp[:r, ps_, nf, :, 0:64], SV[t0:t0 + r, p * 128:(p + 1) * 128].rearrange("q (h d) -> q h d", h=2), writes=[("Vp", ps_)])
                self.load_tiled(Op, ps_, SO, p * 128, 128, "Op")
                for g0 in range(0, ntile, 4):
                    qis = list(range(g0, min(g0 + 4, ntile)))
                    q_end = self.tiles[qis[-1]][0] + self.tiles[qis[-1]][1]
                    for qi in qis:
                        pass
                    for kb in range(0, qis[-1] + 1):
                        k0, rk = self.tiles[kb]
                        qlo = max(kb, g0)
                        c0 = self.tiles[qlo][0]
                        N = q_end - c0
                        for hh in range(2):
                            sb_ = scnt % 4
                            scnt += 1
                            ps_s = self.psa[sb_]
                            self.op("pe", lambda e, hh=hh, ps_=ps_, k0=k0, rk=rk, c0=c0, N=N, ps_s=ps_s: e.matmul(ps_s[:rk, 0:N], lhsT=KT[hh * 64:(hh + 1) * 64, ps_, k0:k0 + rk], rhs=QT[hh * 64:(hh + 1) * 64, ps_, c0:c0 + N], start=True, stop=True),
                                    reads=[("KT", ps_), ("QT", ps_)], writes=[("pa", sb_)])
                            for qi in range(qlo, qis[-1] + 1):
                                tq0, rq = self.tiles[qi]
                                off = tq0 - c0
                                ql = qi - g0
                                bs = bcnt % 2
                                bcnt += 1
                                hcol = 2 * p + hh
                                self.op("dve", lambda e, rk=rk, bs=bs, kb=kb, qi=qi, hcol=hcol: e.tensor_tensor(out=bias[:rk, bs, 0, 0:1], in0=NC[:rk, kb, hcol:hcol + 1], in1=NR[:rk, qi, hcol:hcol + 1], op=ALU.subtract),
                                        reads=[("NC", kb), ("NR", qi)], writes=[("bias", bs)])
                                self.op("act", lambda e, rk=rk, rq=rq, off=off, sb_=sb_, ps_s=ps_s, bs=bs: e.activation(out=P[:rk, sb_, off:off + rq], in_=ps_s[:rk, off:off + rq], func=AF.Exp, bias=bias[:rk, bs, 0, 0:1]),
                                        reads=[("pa", sb_), ("bias", bs)], writes=[("P", sb_)])
                                if qi == kb:
                                    self.op("dve", lambda e, rk=rk, rq=rq, off=off, sb_=sb_: e.tensor_tensor(out=P[:rk, sb_, off:off + rq], in0=P[:rk, sb_, off:off + rq], in1=self.mask_bf[:rk, 0:rq], op=ALU.mult),
                                            reads=[("P", sb_), "mask_bf"], writes=[("P", sb_)])
                                po = self.psa[4 + hh]
                                self.op("pe", lambda e, rk=rk, rq=rq, off=off, sb_=sb_, po=po, ql=ql, kb=kb, qi=qi, hh=hh, ps_=ps_, g0=g0: e.matmul(po[:rq, ql * 65:(ql + 1) * 65], lhsT=P[:rk, sb_, off:off + rq], rhs=Vp[:rk, ps_, kb, hh, :], start=(kb == 0 and qi == g0), stop=(kb == qi)),
                                        reads=[("P", sb_), ("Vp", ps_)], writes=[("pa", 4 + hh)])
                    for qi in qis:
                        tq0, rq = self.tiles[qi]
                        ql = qi - g0
                        for hh in range(2):
                            po = self.psa[4 + hh]
                            self.op("dve", lambda e, rq=rq, ql=ql, hh=hh, po=po: e.reciprocal(out=rd[:rq, ql, hh:hh + 1], in_=po[:rq, ql * 65 + 64:ql * 65 + 65]), reads=[("pa", 4 + hh)], writes=[("rd", ql)])
                            self.op("dve", lambda e, rq=rq, ql=ql, hh=hh, po=po, qi=qi, ps_=ps_: e.scalar_tensor_tensor(out=Yp[:rq, ps_, qi, hh * 64:(hh + 1) * 64], in0=po[:rq, ql * 65:ql * 65 + 64], scalar=rd[:rq, ql, hh:hh + 1],
                                                                                                                        in1=Op[:rq, ps_, qi, hh * 64:(hh + 1) * 64], op0=ALU.mult, op1=ALU.mult),
                                    reads=[("pa", 4 + hh), ("rd", ql), ("Op", ps_)], writes=[("Yp", ps_)])
                self.store_tiled(SY, p * 128, 128, Yp, ps_, "Yp")
        self.p.barrier()
        self.out_proj(t["fox_w_out"][j])

    def L_final(self):
        t = self.t
        H, out = t["H"], t["out"]
        with contextlib.ExitStack() as st:
            gam = self.bcast_row(st, "gamf", t["final_norm"][0:1, :], D)
            hs = self.sb(st, "hsf", [128, 2, D], F32)
            ot = self.sb(st, "otf", [128, 2, D], F32)
            ss = self.sb(st, "ssf", [128, 2, 4], F32)
            junk = self.sb(st, "junkf", [128, D], BF16)
            for i, (t0, r) in enumerate(self.tiles):
                s = i % 2
                self.dma("sp", hs[:r, s, :], H[t0:t0 + r, :], reads=[("H", i)], writes=[("hsf", s)])
                self.op("act", lambda e, r=r, s=s: e.activation(out=junk[:r, :], in_=hs[:r, s, :], func=AF.Square, accum_out=ss[:r, s, 0:1]),
                        reads=[("hsf", s)], writes=["junkf", ("ssf", s)])
                self.rstd(ss[:r, s, 0:1], ss[:r, s, 1:2], ss[:r, s, 2:3], 1.0 / D, [("ssf", s)])
                self.op("dve", lambda e, r=r, s=s: e.scalar_tensor_tensor(out=ot[:r, s, :], in0=hs[:r, s, :], scalar=ss[:r, s, 2:3], in1=gam[:r, :], op0=ALU.mult, op1=ALU.mult),
                        reads=[("hsf", s), ("ssf", s), "gamf"], writes=[("otf", s)])
                lo = N_META if i == 0 else 0
                self.dma("sp", out[t0 + lo - N_META:t0 + r - N_META, :], ot[lo:r, s, :], reads=[("otf", s)])


def build_nc(NT, layers, final=True):
    nc = bass.Bass("TRN2", target_bir_lowering=False)
    kb = KB(nc, NT, layers, final)
    kb.build()
    return nc, kb


def make_consts():
    c = np.zeros((128, 640), np.float32)
    c[:, 0:128] = np.eye(128)
    c[:, 128:256] = np.triu(np.ones((128, 128)))
    c[:, 256:384] = 1.0
    c[64, 384:512] = 1.0
    c[8, 512:640] = 1.0
    return c


ALL_LAYERS = [("ml", 0), ("ffn", 0), ("fox", 0), ("moe", 0), ("ml", 1), ("ffn", 1), ("fox", 1), ("moe", 1)]
NCORES = 4


def kernel(**inputs):
    B, S, _ = inputs["x"].shape
    assert B == 4 and S == 4096
    nc, kb = build_nc(S + N_META, ALL_LAYERS)
    shared = {}
    for name in kb.used_inputs:
        if name == "x":
            continue
        if name == "consts":
            shared[name] = make_consts()
        elif name == "final_norm":
            shared[name] = np.ascontiguousarray(inputs[name], dtype=np.float32).reshape(1, D)
        else:
            shared[name] = np.ascontiguousarray(inputs[name], dtype=np.float32)
    x = np.ascontiguousarray(inputs["x"], dtype=np.float32)
    in_maps = []
    for b in range(B):
        m = dict(shared)
        m["x"] = x[b]
        in_maps.append(m)
    res = run_bass_kernel_spmd(nc, in_maps, core_ids=list(range(NCORES)))
    return np.stack([res.results[b]["out"] for b in range(B)], axis=0).astype(np.float32)
```

```python
import numpy as np
import concourse.bass as bass
import concourse.mybir as mybir
from concourse.bass_utils import run_bass_kernel_spmd

F32 = mybir.dt.float32
BF16 = mybir.dt.bfloat16
AF = mybir.ActivationFunctionType
ALU = mybir.AluOpType
AX = mybir.AxisListType

D = 2048
KC = 16
N_META = 16
EPS = 1e-6
ML_H = 8
ML_DQK = 128
ML_DV = 256
ML_IN = 6160
FOX_H = 32
FOX_DH = 64
FOX_IN = 8224
D_FF = 5632
FC = 44
N_EXP = 8
GATE_CAP = 15.0


class _Op:
    __slots__ = ("eng", "idx", "fn", "deps", "signal", "is_dma", "sem", "val", "pre", "phase")

    def __init__(self, eng, idx, fn, is_dma):
        self.eng = eng
        self.idx = idx
        self.fn = fn
        self.deps = []
        self.signal = False
        self.is_dma = is_dma
        self.sem = None
        self.val = None
        self.pre = None


class Prog:
    COMPUTE = ("pe", "act", "dve", "pool")
    QUEUES = ("sp", "act", "pool")
    NWIN = 8

    def __init__(self, nc):
        self.nc = nc
        self.ops = {e: [] for e in ("pe", "act", "dve", "pool", "sp")}
        self.res = {}
        self.ndma = {q: 0 for q in self.QUEUES}
        self.dma_ops = {q: [] for q in self.QUEUES}
        self.same_engine_sync = True
        self.phase = 0

    def _states(self, tok, create):
        name, sub = tok if isinstance(tok, tuple) else (tok, None)
        d = self.res.setdefault(name, {})
        if sub is None:
            if None not in d:
                d[None] = [None, []]
            return list(d.values())
        out = []
        if sub not in d:
            d[sub] = [None, []]
            if None in d:
                d[sub][0] = d[None][0]
                d[sub][1] = list(d[None][1])
        out.append(d[sub])
        return out

    def add(self, eng, fn, reads=(), writes=(), dma=False):
        op = _Op(eng, len(self.ops[eng]), fn, dma)
        op.phase = self.phase
        deps = set()
        for tok in reads:
            for st in self._states(tok, True):
                if st[0] is not None:
                    deps.add(st[0])
        for tok in writes:
            for st in self._states(tok, True):
                if st[0] is not None:
                    deps.add(st[0])
                for r in st[1]:
                    deps.add(r)
        for tok in reads:
            name, sub = tok if isinstance(tok, tuple) else (tok, None)
            d = self.res[name]
            if sub is None:
                for st in d.values():
                    st[1].append(op)
            else:
                d[sub][1].append(op)
        for tok in writes:
            name, sub = tok if isinstance(tok, tuple) else (tok, None)
            d = self.res[name]
            if sub is None:
                for k in list(d.keys()):
                    d[k] = [op, []]
            else:
                d[sub] = [op, []]
                if None in d:
                    pass
        deps.discard(op)
        op.deps = self._dedup(deps)
        self.ops[eng].append(op)
        if dma:
            q = eng
            op.pre = None
            n = self.ndma[q]
            self.ndma[q] += 1
            self.dma_ops[q].append(op)
            op.sem = (q, n % self.NWIN)
            op.val = 16 * (n // self.NWIN + 1)
            if n >= self.NWIN:
                op.pre = self.dma_ops[q][n - self.NWIN]
        return op

    @staticmethod
    def _dedup(deps):
        best = {}
        out = []
        for d in deps:
            if d.is_dma:
                out.append(d)
            else:
                b = best.get(d.eng)
                if b is None or d.idx > b.idx:
                    best[d.eng] = d
        out.extend(best.values())
        return out

    def emit(self):
        nc = self.nc
        for e, lst in self.ops.items():
            for op in lst:
                for d in op.deps:
                    if d.is_dma:
                        continue
                    if d.eng == op.eng and not op.is_dma:
                        if d.eng == "pe" or not self.same_engine_sync:
                            continue
                    d.signal = True
        for e in self.COMPUTE:
            c = 0
            ph = -1
            for op in self.ops[e]:
                if op.is_dma:
                    continue
                if op.phase != ph:
                    ph = op.phase
                    c = 0
                if op.signal:
                    c += 1
                    op.val = c
                    assert c < 30000, (e, ph, c)
        for q in self.QUEUES:
            for op in self.dma_ops[q]:
                assert op.val < 30000, (q, op.val)
        import contextlib
        with contextlib.ExitStack() as st:
            sems = {}
            for e in self.COMPUTE:
                for ph in sorted(set(op.phase for op in self.ops[e] if not op.is_dma)):
                    sems[(e, ph)] = st.enter_context(nc.semaphore("s_%s%d" % (e, ph)))
            for q in self.QUEUES:
                if not self.ndma[q]:
                    continue
                for i in range(self.NWIN):
                    sems[(q, i)] = st.enter_context(nc.semaphore("d_%s%d" % (q, i)))
            blk = st.enter_context(nc.Block())
            prog = self

            def run(ename, eobj):
                waited = {}
                waited_dma = set()
                for op in prog.ops[ename]:
                    waits = []
                    if op.is_dma and op.pre is not None:
                        waits.append(op.pre)
                    waits.extend(op.deps)
                    for d in waits:
                        if d.is_dma:
                            if id(d) in waited_dma:
                                continue
                            waited_dma.add(id(d))
                            eobj.wait_ge(sems[d.sem], d.val)
                        else:
                            if d.eng == ename and not op.is_dma:
                                if ename == "pe" or not prog.same_engine_sync:
                                    continue
                            if waited.get((d.eng, d.phase), 0) >= d.val:
                                continue
                            waited[(d.eng, d.phase)] = d.val
                            eobj.wait_ge(sems[(d.eng, d.phase)], d.val)
                    ins = op.fn(eobj)
                    if op.is_dma:
                        ins.then_inc(sems[op.sem], 16)
                    elif op.signal:
                        ins.then_inc(sems[(ename, op.phase)], 1)
                if ename in prog.QUEUES:
                    n = prog.ndma[ename]
                    for i in range(min(n, prog.NWIN)):
                        last = None
                        for op in reversed(prog.dma_ops[ename]):
                            if op.sem == (ename, i):
                                last = op
                                break
                        if last is not None:
                            eobj.wait_ge(sems[last.sem], last.val)

            @blk.tensor
            def _(e):
                run("pe", e)

            @blk.scalar
            def _(e):
                run("act", e)

            @blk.vector
            def _(e):
                run("dve", e)

            @blk.gpsimd
            def _(e):
                run("pool", e)

            @blk.sync
            def _(e):
                run("sp", e)

    def barrier(self):
        lasts = []
        for e in self.COMPUTE:
            for op in reversed(self.ops[e]):
                if not op.is_dma:
                    lasts.append(op)
                    break
        for q in self.QUEUES:
            seen = set()
            for op in reversed(self.dma_ops[q]):
                if op.sem not in seen:
                    seen.add(op.sem)
                    lasts.append(op)
                if len(seen) == self.NWIN:
                    break
        self.res = {}
        self._bar = lasts
        self.phase += 1

    def add_b(self, eng, fn, reads=(), writes=(), dma=False):
        op = self.add(eng, fn, reads, writes, dma)
        bar = getattr(self, "_bar", None)
        if bar:
            key = "_bdone_" + eng
            if getattr(self, key, None) is not bar:
                setattr(self, key, bar)
                s = set(op.deps)
                for b in bar:
                    if b is not op:
                        s.add(b)
                op.deps = self._dedup(s)
        return op


def _cdiv(a, b):
    return (a + b - 1) // b


import contextlib


class KB:
    def __init__(self, nc, NT, layers, final=True, split=False, rgroups=None):
        self.nc = nc
        self.layers = layers
        self.final = final
        self.split = split
        self.rgroups = rgroups
        self.HS = 2 if split else 1
        self.NTO = NT
        self.NTF = 2 * NT if split else NT
        self.NX = NT if split else NT - N_META
        self.p = Prog(nc)
        self.set_tokens(NT)
        self.wslot = 0
        self.SH = dict(self.SHAPES)
        if split:
            L = self.SHAPES["ml_norm"][0]
            self.SH.update({"ml_w_in": (L, D, 3080), "ml_b_i": (L, 4), "ml_b_f": (L, 4), "ml_h_gain": (L, 1024), "ml_w_out": (L, 1024, D),
                            "fox_w_in": (L, D, 4112), "fox_b_f": (L, 16), "fox_w_out": (L, 1024, D)})

    def set_tokens(self, NT):
        self.NT = NT
        self.tiles = [(t0, min(128, NT - t0)) for t0 in range(0, NT, 128)]
        self.ntile = len(self.tiles)
        self.nfull = NT // 128

    def op(self, eng, fn, reads=(), writes=()):
        return self.p.add_b(eng, fn, reads, writes, False)

    def dma(self, q, out, in_, reads=(), writes=()):
        return self.p.add_b(q, lambda e: e.dma_start(out=out, in_=in_), reads, writes, True)

    SHAPES = {
        "meta_tokens": (N_META, D),
        "ml_norm": (2, D), "ml_w_in": (2, D, ML_IN), "ml_b_i": (2, 8), "ml_b_f": (2, 8),
        "ml_h_gain": (2, D), "ml_w_out": (2, D, D),
        "ffn_norm": (2, D), "ffn_w_gate": (2, D, D_FF), "ffn_w_up": (2, D, D_FF), "ffn_w_down": (2, D_FF, D),
        "fox_norm": (2, D), "fox_w_in": (2, D, FOX_IN), "fox_b_f": (2, 32),
        "fox_q_gain": (2, 64), "fox_k_gain": (2, 64), "fox_w_out": (2, D, D),
        "moe_norm": (2, D), "moe_router": (2, D, 8),
        "moe_w_gate": (2, 8, D, D_FF), "moe_w_up": (2, 8, D, D_FF), "moe_w_down": (2, 8, D_FF, D),
        "final_norm": (1, D), "consts": (128, 5 * 128),
    }

    def declare(self):
        nc = self.nc
        NT, NX = self.NTF, self.NX
        kb = self

        class Lazy(dict):
            def __missing__(self, name):
                shape = (NX, D) if name == "x" else kb.SH[name]
                ap = nc.dram_tensor(name, list(shape), F32, kind="ExternalInput").ap()
                self[name] = ap
                kb.used_inputs.append(name)
                return ap

        self.used_inputs = []
        t = Lazy()
        t["out"] = nc.dram_tensor("out", [NX, D], F32, kind="ExternalOutput").ap()
        t["H"] = nc.dram_tensor("H", [self.NTO, D], F32).ap()
        if self.split:
            t["Hfull"] = nc.dram_tensor("Hfull", [NT, D], F32).ap()
            t["PD"] = nc.dram_tensor("PD", [NT, D], F32).ap()
            t["DO"] = nc.dram_tensor("DO", [self.NTO, D], F32).ap()
        else:
            t["Hfull"] = t["H"]
        t["S_QT"] = nc.dram_tensor("S_QT", [16, 128, NT], BF16).ap()
        t["S_KT"] = nc.dram_tensor("S_KT", [16, 128, NT], BF16).ap()
        t["S_K"] = nc.dram_tensor("S_K", [NT, 1024], BF16).ap()
        t["S_V"] = nc.dram_tensor("S_V", [NT, D], BF16).ap()
        t["S_O"] = nc.dram_tensor("S_O", [NT, D], BF16).ap()
        t["S_Y"] = nc.dram_tensor("S_Y", [NT, D], BF16).ap()
        t["S_G"] = nc.dram_tensor("S_G", [NT, 32], F32).ap()
        self.t = t

    def sb(self, st, name, shape, dt):
        self._uid = getattr(self, "_uid", 0) + 1
        tl = st.enter_context(self.nc.sbuf_tensor("%s_%d" % (name, self._uid), list(shape), dt))
        self._ln = getattr(self, "_ln", {})
        self._ln[tl.name] = name
        return tl

    def ln(self, tl):
        return self._ln[tl.name]

    def build(self):
        nc = self.nc
        self.declare()
        t = self.t
        with contextlib.ExitStack() as st:
            self.pst = [st.enter_context(nc.psum_tensor("pt%d" % i, [128, 1024], BF16)) for i in range(2)]
            self.psa = [st.enter_context(nc.psum_tensor("pa%d" % i, [128, 512], F32)) for i in range(6)]
            cf = self.sb(st, "cf", [128, 640], F32)
            self.cf = cf
            self.ident_bf = self.sb(st, "ident_bf", [128, 128], BF16)
            self.mask_bf = self.sb(st, "mask_bf", [128, 128], BF16)
            self.dma("sp", cf[:, :], t["consts"][:, :], writes=["cf"])
            self.op("dve", lambda e: e.tensor_copy(out=self.ident_bf[:, :], in_=cf[:, 0:128]), reads=["cf"], writes=["ident_bf"])
            self.op("dve", lambda e: e.tensor_copy(out=self.mask_bf[:, :], in_=cf[:, 128:256]), reads=["cf"], writes=["mask_bf"])
            self.tri_f = cf[:, 128:256]
            self.ones_f = cf[:, 256:384]
            self.sel64 = cf[:, 384:512]
            self.sel8 = cf[:, 512:640]
            self.Hsrc = t["H"]
            if self.split:
                self.dma("sp", t["H"][:, :], t["x"][:, :])
            else:
                self.dma("sp", t["H"][0:N_META, :], t["meta_tokens"][:, :])
                self.dma("sp", t["H"][N_META:self.NT, :], t["x"][:, :])
            self.p.barrier()
            for kind, j in self.layers:
                getattr(self, "L_" + kind)(j)
                self.p.barrier()
            if self.final:
                self.L_final()
            self.p.emit()

    def bcast_row(self, st, name, src_row_ap, n, q="sp"):
        tl = self.sb(st, name, [128, n], F32)
        self.dma(q, tl[:, :], src_row_ap.partition_broadcast(128), writes=[name])
        return tl

    def rstd(self, ssum, tmp, out, inv_n, toks):
        self.op("dve", lambda e: e.tensor_scalar(out=tmp, in0=ssum, scalar1=inv_n, scalar2=EPS, op0=ALU.mult, op1=ALU.add), reads=toks, writes=toks)
        self.op("act", lambda e: e.activation(out=tmp, in_=tmp, func=AF.Sqrt), reads=toks, writes=toks)
        self.op("dve", lambda e: e.reciprocal(out=out, in_=tmp), reads=toks, writes=toks)

    def load_norm_T(self, bufs, i, gamma, xT, col0, it):
        t0, r = self.tiles[i]
        s = it % 2
        hs, xn, ss, junk = bufs["hs"], bufs["xn"], bufs["ss"], bufs["junk"]
        H = self.Hsrc
        self.dma("sp", hs[:r, s, :], H[t0:t0 + r, :], reads=[("H", i)], writes=[("hs", s)])
        self.op("act", lambda e: e.activation(out=junk[:r, :], in_=hs[:r, s, :], func=AF.Square, accum_out=ss[:r, s, 0:1]),
                reads=[("hs", s)], writes=["junk", ("ss", s)])
        self.rstd(ss[:r, s, 0:1], ss[:r, s, 1:2], ss[:r, s, 2:3], 1.0 / D, [("ss", s)])
        self.op("dve", lambda e: e.scalar_tensor_tensor(out=xn[:r, s, :], in0=hs[:r, s, :], scalar=ss[:r, s, 2:3], in1=gamma[:r, :], op0=ALU.mult, op1=ALU.mult),
                reads=[("hs", s), ("ss", s), self.ln(gamma)], writes=[("xn", s)])
        self.transpose_into(xn, s, r, xT, col0, "xn")
        return s

    def transpose_into(self, src, s, r, xT, col0, srcname, nch=16):
        for half in range(nch // 8):
            pt = self.pst[half]
            for k in range(8):
                kc = half * 8 + k
                self.op("pe", lambda e, kc=kc, k=k, pt=pt: e.transpose(out=pt[:, k * 128:k * 128 + r], in_=src[:r, s, kc * 128:(kc + 1) * 128], identity=self.ident_bf[:r, :r]),
                        reads=[(srcname, s), "ident_bf"], writes=[("pt", half)])
            eng = "act" if half == 0 else "dve"
            src_ap = pt[:, :].rearrange("p (k c) -> p k c", k=8)[:, :, 0:r]
            dst_ap = xT[:, half * 8:(half + 1) * 8, col0:col0 + r]
            if eng == "act":
                self.op("act", lambda e, a=dst_ap, b=src_ap: e.copy(out=a, in_=b), reads=[("pt", half)], writes=[(self.ln(xT), col0 // 128)])
            else:
                self.op("dve", lambda e, a=dst_ap, b=src_ap: e.tensor_copy(out=a, in_=b), reads=[("pt", half)], writes=[(self.ln(xT), col0 // 128)])

    def wload(self, wt, nslots, src3, ncols, name):
        s = self.wslot_of.setdefault(wt.name, 0)
        self.wslot_of[wt.name] = (s + 1) % nslots
        kcn = src3.shape[1]
        self.dma("pool", wt[:, s, 0:kcn, 0:ncols], src3, writes=[(name, s)])
        return s

    wslot_of = {}

    GF = 6

    def L_ffn(self, j):
        t = self.t
        self._ffn(t["ffn_norm"][j:j + 1, :], [(t["ffn_w_gate"][j], t["ffn_w_up"][j], t["ffn_w_down"][j])], None)

    def L_moe(self, j):
        t = self.t
        ws = [(t["moe_w_gate"][j, e], t["moe_w_up"][j, e], t["moe_w_down"][j, e]) for e in range(N_EXP)]
        self._ffn(t["moe_norm"][j:j + 1, :], ws, t["moe_router"][j])

    def _ffn(self, gamma_row, weights, router):
        nc = self.nc
        H = self.t["H"]
        GF = self.GF
        GT = GF * 128
        with contextlib.ExitStack() as st:
            gam = self.bcast_row(st, "gam", gamma_row, D)
            bufs = {"hs": self.sb(st, "hs", [128, 2, D], F32), "xn": self.sb(st, "xn", [128, 2, D], BF16),
                    "ss": self.sb(st, "ss", [128, 2, 4], F32), "junk": self.sb(st, "junk", [128, D], BF16)}
            xT = self.sb(st, "xT", [128, KC, GT], BF16)
            aT = self.sb(st, "aT", [128, FC, GT], BF16)
            wg = self.sb(st, "wg", [128, 2, KC, 256], BF16)
            wu = self.sb(st, "wu", [128, 2, KC, 256], BF16)
            wd = self.sb(st, "wd", [128, 2, 11, 512], BF16)
            sg = self.sb(st, "sg", [128, 2, 512], F32)
            hr = self.sb(st, "hr", [128, 2, 512], F32)
            ho = self.sb(st, "ho", [128, 2, 512], F32)
            if router is not None:
                rw = self.sb(st, "rw", [128, KC, 8], BF16)
                self.dma("pool", rw[:, :, :], router.rearrange("(kc p) e -> p kc e", p=128), writes=["rw"])
                comb = self.sb(st, "comb", [128, GF, 8], F32)
                rt = self.sb(st, "rt", [128, GF, 40], F32)
            it = 0
            epi = 0
            for g0 in range(0, self.ntile, GF):
                gt = list(range(g0, min(g0 + GF, self.ntile)))
                ntok = sum(self.tiles[i][1] for i in gt)
                for li, i in enumerate(gt):
                    self.load_norm_T(bufs, i, gam, xT, li * 128, it)
                    it += 1
                if router is not None:
                    for li, i in enumerate(gt):
                        r = self.tiles[i][1]
                        ps = self.psa[li % 2]
                        for kc in range(KC):
                            self.op("pe", lambda e, kc=kc, li=li, r=r, ps=ps: e.matmul(ps[:r, 0:8], lhsT=xT[:, kc, li * 128:li * 128 + r], rhs=rw[:, kc, :], start=(kc == 0), stop=(kc == KC - 1)),
                                    reads=[("xT", li), "rw"], writes=[("pa", li % 2)])
                        L = rt[:r, li, 0:8]; m1 = rt[:r, li, 8:9]; eq = rt[:r, li, 16:24]; l2 = rt[:r, li, 24:32]; m2 = rt[:r, li, 9:10]
                        ex = rt[:r, li, 32:40]; dn = rt[:r, li, 10:11]; ge = rt[:r, li, 16:24]
                        tk = [("rt", li)]
                        self.op("dve", lambda e, L=L, ps=ps, r=r: e.tensor_copy(out=L, in_=ps[:r, 0:8]), reads=[("pa", li % 2)], writes=tk)
                        self.op("dve", lambda e, L=L, m1=m1: e.tensor_reduce(out=m1, in_=L, axis=AX.X, op=ALU.max), reads=tk, writes=tk)
                        self.op("dve", lambda e, L=L, m1=m1, eq=eq: e.tensor_scalar(out=eq, in0=L, scalar1=m1, scalar2=-1e30, op0=ALU.is_ge, op1=ALU.mult), reads=tk, writes=tk)
                        self.op("dve", lambda e, L=L, eq=eq, l2=l2: e.tensor_tensor(out=l2, in0=L, in1=eq, op=ALU.add), reads=tk, writes=tk)
                        self.op("dve", lambda e, l2=l2, m2=m2: e.tensor_reduce(out=m2, in_=l2, axis=AX.X, op=ALU.max), reads=tk, writes=tk)
                        self.op("dve", lambda e, L=L, m1=m1, l2=l2: e.tensor_scalar(out=l2, in0=L, scalar1=m1, scalar2=None, op0=ALU.subtract), reads=tk, writes=tk)
                        self.op("act", lambda e, l2=l2, ex=ex: e.activation(out=ex, in_=l2, func=AF.Exp), reads=tk, writes=tk)
                        self.op("dve", lambda e, L=L, m2=m2, ge=ge: e.tensor_scalar(out=ge, in0=L, scalar1=m2, scalar2=None, op0=ALU.is_ge), reads=tk, writes=tk)
                        self.op("dve", lambda e, ex=ex, ge=ge: e.tensor_tensor(out=ex, in0=ex, in1=ge, op=ALU.mult), reads=tk, writes=tk)
                        self.op("dve", lambda e, ex=ex, dn=dn: e.tensor_reduce(out=dn, in_=ex, axis=AX.X, op=ALU.add), reads=tk, writes=tk)
                        self.op("dve", lambda e, dn=dn: e.reciprocal(out=dn, in_=dn), reads=tk, writes=tk)
                        self.op("dve", lambda e, ex=ex, dn=dn, li=li, r=r: e.tensor_scalar(out=comb[:r, li, :], in0=ex, scalar1=dn, scalar2=None, op0=ALU.mult), reads=tk, writes=[("comb", li)])
                ctl = [(c0, min(512, ntok - c0)) for c0 in range(0, ntok, 512)]
                for ei, (Wg, Wu, Wd) in enumerate(weights):
                    Wg3 = Wg.rearrange("(kc p) f -> p kc f", p=128)
                    Wu3 = Wu.rearrange("(kc p) f -> p kc f", p=128)
                    Wd3 = Wd.rearrange("(fc p) d -> p fc d", p=128)
                    step = 0
                    for fg in range(FC // 2):
                        sgs = self.wload(wg, 2, Wg3[:, :, fg * 256:(fg + 1) * 256], 256, "wg")
                        sus = self.wload(wu, 2, Wu3[:, :, fg * 256:(fg + 1) * 256], 256, "wu")
                        for (c0, N) in ctl:
                            for fc in range(2):
                                b = (step % 2) * 2
                                step += 1
                                pg, pu = self.psa[b], self.psa[b + 1]
                                ctoks = [("xT", c) for c in range(c0 // 128, _cdiv(c0 + N, 128))]
                                for kc in range(KC):
                                    self.op("pe", lambda e, kc=kc, pg=pg, fc=fc, c0=c0, N=N, sgs=sgs: e.matmul(pg[:, 0:N], lhsT=wg[:, sgs, kc, fc * 128:(fc + 1) * 128], rhs=xT[:, kc, c0:c0 + N], start=(kc == 0), stop=(kc == KC - 1)),
                                            reads=[("wg", sgs)] + ctoks, writes=[("pa", b)])
                                for kc in range(KC):
                                    self.op("pe", lambda e, kc=kc, pu=pu, fc=fc, c0=c0, N=N, sus=sus: e.matmul(pu[:, 0:N], lhsT=wu[:, sus, kc, fc * 128:(fc + 1) * 128], rhs=xT[:, kc, c0:c0 + N], start=(kc == 0), stop=(kc == KC - 1)),
                                            reads=[("wu", sus)] + ctoks, writes=[("pa", b + 1)])
                                ss_ = step % 2
                                self.op("act", lambda e, pg=pg, N=N, ss_=ss_: e.activation(out=sg[:, ss_, 0:N], in_=pg[:, 0:N], func=AF.Silu),
                                        reads=[("pa", b)], writes=[("sg", ss_)])
                                fch = fg * 2 + fc
                                self.op("dve", lambda e, pu=pu, N=N, ss_=ss_, fch=fch, c0=c0: e.tensor_tensor(out=aT[:, fch, c0:c0 + N], in0=sg[:, ss_, 0:N], in1=pu[:, 0:N], op=ALU.mult),
                                        reads=[("sg", ss_), ("pa", b + 1)], writes=[("aT", fch)])
                    for dt in range(4):
                        for q4 in range(4):
                            sd = self.wload(wd, 2, Wd3[:, q4 * 11:(q4 + 1) * 11, dt * 512:(dt + 1) * 512], 512, "wd")
                            for jj in range(11):
                                fch = q4 * 11 + jj
                                for li, i in enumerate(gt):
                                    r = self.tiles[i][1]
                                    self.op("pe", lambda e, li=li, r=r, fch=fch, jj=jj, sd=sd: e.matmul(self.psa[li][:r, :], lhsT=aT[:, fch, li * 128:li * 128 + r], rhs=wd[:, sd, jj, :], start=(fch == 0), stop=(fch == FC - 1)),
                                            reads=[("aT", fch), ("wd", sd)], writes=[("pa", li)])
                        for li, i in enumerate(gt):
                            t0, r = self.tiles[i]
                            s = epi % 2
                            epi += 1
                            self.dma("sp", hr[:r, s, :], H[t0:t0 + r, dt * 512:(dt + 1) * 512], reads=[("H", i)], writes=[("hr", s)])
                            if router is None:
                                self.op("dve", lambda e, li=li, r=r, s=s: e.tensor_tensor(out=ho[:r, s, :], in0=self.psa[li][:r, :], in1=hr[:r, s, :], op=ALU.add),
                                        reads=[("pa", li), ("hr", s)], writes=[("ho", s)])
                            else:
                                self.op("dve", lambda e, li=li, r=r, s=s, ei=ei: e.scalar_tensor_tensor(out=ho[:r, s, :], in0=self.psa[li][:r, :], scalar=comb[:r, li, ei:ei + 1], in1=hr[:r, s, :], op0=ALU.mult, op1=ALU.add),
                                        reads=[("pa", li), ("hr", s), ("comb", li)], writes=[("ho", s)])
                            self.dma("sp", H[t0:t0 + r, dt * 512:(dt + 1) * 512], ho[:r, s, :], reads=[("ho", s)], writes=[("H", i)])

    GP = 4

    def groups(self):
        for g0 in range(0, self.ntile, self.GP):
            gt = list(range(g0, min(g0 + self.GP, self.ntile)))
            ntok = sum(self.tiles[i][1] for i in gt)
            yield gt, self.tiles[g0][0], ntok

    def a_style(self, xT, w, ws, li, r, ps, ncols, wname, nkc=KC):
        for kc in range(nkc):
            self.op("pe", lambda e, kc=kc: e.matmul(ps[:r, 0:ncols], lhsT=xT[:, kc, li * 128:li * 128 + r], rhs=w[:, ws, kc, 0:ncols], start=(kc == 0), stop=(kc == nkc - 1)),
                    reads=[(self.ln(xT), li), (wname, ws)], writes=[("pa", self._psidx(ps))])

    def b_style(self, xT, w, ws, c0, ps, ntok, gt, wname):
        toks = [(self.ln(xT), li) for li in range(len(gt))]
        for kc in range(KC):
            self.op("pe", lambda e, kc=kc: e.matmul(ps[:, 0:ntok], lhsT=w[:, ws, kc, c0:c0 + 128], rhs=xT[:, kc, 0:ntok], start=(kc == 0), stop=(kc == KC - 1)),
                    reads=toks + [(wname, ws)], writes=[("pa", self._psidx(ps))])

    def _psidx(self, ps):
        for i, p in enumerate(self.psa):
            if p is ps:
                return i
        raise KeyError

    def evac(self, k, out, in_, reads, writes, func=None, scale=1.0):
        if func is not None or k % 2 == 0:
            f = func if func is not None else AF.Copy
            self.op("act", lambda e: e.activation(out=out, in_=in_, func=f, scale=scale), reads=reads, writes=writes)
        else:
            if scale == 1.0:
                self.op("dve", lambda e: e.tensor_copy(out=out, in_=in_), reads=reads, writes=writes)
            else:
                self.op("dve", lambda e: e.tensor_scalar(out=out, in0=in_, scalar1=scale, scalar2=None, op0=ALU.mult), reads=reads, writes=writes)

    def out_proj(self, W):
        H, SY = self.t["H"], self.t["S_Y"]
        W3 = W.rearrange("(kc p) f -> p kc f", p=128)
        nkc = KC // self.HS
        YW = D // self.HS
        with contextlib.ExitStack() as st:
            yt = self.sb(st, "yt", [128, 2, YW], BF16)
            xT = self.sb(st, "xT", [128, KC, self.GP * 128], BF16)
            w = self.sb(st, "w", [128, 2, KC, 512], BF16)
            hr = self.sb(st, "hr", [128, 2, 512], F32)
            ho = self.sb(st, "ho", [128, 2, 512], F32)
            it = 0
            epi = 0
            for gt, tok0, ntok in self.groups():
                for li, i in enumerate(gt):
                    t0, r = self.tiles[i]
                    s = it % 2
                    it += 1
                    self.dma("sp", yt[:r, s, :], SY[t0:t0 + r, 0:YW], writes=[("yt", s)])
                    self.transpose_into(yt, s, r, xT, li * 128, "yt", nch=nkc)
                for dt in range(4):
                    ws = self.wload(w, 2, W3[:, :, dt * 512:(dt + 1) * 512], 512, "w")
                    for li, i in enumerate(gt):
                        t0, r = self.tiles[i]
                        ps = self.psa[(epi % 4)]
                        self.a_style(xT, w, ws, li, r, ps, 512, "w", nkc=nkc)
                        s = epi % 2
                        epi += 1
                        if self.split:
                            self.op("act", lambda e, r=r, s=s, ps=ps: e.copy(out=ho[:r, s, :], in_=ps[:r, :]), reads=[("pa", self._psidx(ps))], writes=[("ho", s)])
                            self.dma("sp", self.t["PD"][t0:t0 + r, dt * 512:(dt + 1) * 512], ho[:r, s, :], reads=[("ho", s)])
                            continue
                        self.dma("sp", hr[:r, s, :], H[t0:t0 + r, dt * 512:(dt + 1) * 512], reads=[("H", i)], writes=[("hr", s)])
                        self.op("dve", lambda e, r=r, s=s, ps=ps: e.tensor_tensor(out=ho[:r, s, :], in0=ps[:r, :], in1=hr[:r, s, :], op=ALU.add),
                                reads=[("pa", self._psidx(ps)), ("hr", s)], writes=[("ho", s)])
                        self.dma("sp", H[t0:t0 + r, dt * 512:(dt + 1) * 512], ho[:r, s, :], reads=[("ho", s)], writes=[("H", i)])
        if self.split:
            self.mixer_end()

    def mixer_begin(self):
        if not self.split:
            return
        t = self.t
        self.p.barrier()
        self.p.add_b("pool", lambda e: e.collective_compute("AllGather", ALU.bypass, replica_groups=self.rgroups, ins=[t["H"][:, :]], outs=[t["Hfull"][:, :]]), (), (), True)
        self.p.barrier()
        self.set_tokens(self.NTF)
        self.Hsrc = t["Hfull"]

    def mixer_end(self):
        t = self.t
        self.p.barrier()
        self.p.add_b("pool", lambda e: e.collective_compute("ReduceScatter", ALU.add, replica_groups=self.rgroups, ins=[t["PD"][:, :]], outs=[t["DO"][:, :]]), (), (), True)
        self.p.barrier()
        self.set_tokens(self.NTO)
        self.Hsrc = t["H"]
        H, DO = t["H"], t["DO"]
        with contextlib.ExitStack() as st:
            a = self.sb(st, "mea", [128, 2, D], F32)
            b = self.sb(st, "meb", [128, 2, D], F32)
            for i, (t0, r) in enumerate(self.tiles):
                s = i % 2
                self.dma("sp", a[:r, s, :], H[t0:t0 + r, :], writes=[("mea", s)])
                self.dma("sp", b[:r, s, :], DO[t0:t0 + r, :], writes=[("meb", s)])
                self.op("dve", lambda e, r=r, s=s: e.tensor_tensor(out=a[:r, s, :], in0=a[:r, s, :], in1=b[:r, s, :], op=ALU.add), reads=[("mea", s), ("meb", s)], writes=[("mea", s)])
                self.dma("sp", H[t0:t0 + r, :], a[:r, s, :], reads=[("mea", s)])

    def load_tiled(self, dst, slot, src, c0, ncols, name):
        nf = self.nfull
        if nf:
            self.dma("sp", dst[:, slot, 0:nf, 0:ncols], src[0:nf * 128, c0:c0 + ncols].rearrange("(i p) c -> p i c", p=128), writes=[(name, slot)])
        if self.ntile > nf:
            t0, r = self.tiles[nf]
            self.dma("sp", dst[:r, slot, nf, 0:ncols], src[t0:t0 + r, c0:c0 + ncols], writes=[(name, slot)])

    def store_tiled(self, dst, c0, ncols, src, slot, name):
        nf = self.nfull
        if nf:
            self.dma("sp", dst[0:nf * 128, c0:c0 + ncols].rearrange("(i p) c -> p i c", p=128), src[:, slot, 0:nf, 0:ncols], reads=[(name, slot)])
        if self.ntile > nf:
            t0, r = self.tiles[nf]
            self.dma("sp", dst[t0:t0 + r, c0:c0 + ncols], src[:r, slot, nf, 0:ncols], reads=[(name, slot)])

    def L_ml(self, j):
        t = self.t
        self.mixer_begin()
        NT, ntile = self.NT, self.ntile
        HS = self.HS
        nh = ML_H // HS
        QW, VW = 1024 // HS, 2048 // HS
        Win = t["ml_w_in"][j].rearrange("(kc p) f -> p kc f", p=128)
        SQT, SKT, SK, SV, SO, SG, SY = t["S_QT"], t["S_KT"], t["S_K"], t["S_V"], t["S_O"], t["S_G"], t["S_Y"]
        with contextlib.ExitStack() as st:
            gam = self.bcast_row(st, "gam", t["ml_norm"][j:j + 1, :], D)
            bufs = {"hs": self.sb(st, "hs", [128, 2, D], F32), "xn": self.sb(st, "xn", [128, 2, D], BF16),
                    "ss": self.sb(st, "ss", [128, 2, 4], F32), "junk": self.sb(st, "junk", [128, D], BF16)}
            xT = self.sb(st, "xT", [128, KC, self.GP * 128], BF16)
            w = self.sb(st, "w", [128, 3, KC, 512], BF16)
            sg = self.sb(st, "stg", [128, 4, 512], BF16)
            sgf = self.sb(st, "stgf", [128, 2, 16], F32)
            it = 0
            ev = 0
            for gt, tok0, ntok in self.groups():
                for li, i in enumerate(gt):
                    self.load_norm_T(bufs, i, gam, xT, li * 128, it)
                    it += 1
                for sec, ntl in (("q", 2 // HS), ("k", 2 // HS), ("v", 4 // HS), ("o", 4 // HS), ("g", 1)):
                    base = {"q": 0, "k": QW, "v": 2 * QW, "o": 2 * QW + VW, "g": 2 * QW + 2 * VW}[sec]
                    for wt in range(ntl):
                        ncols = 2 * nh if sec == "g" else 512
                        ws = self.wload(w, 3, Win[:, :, base + wt * 512:base + wt * 512 + ncols], ncols, "w")
                        if sec in ("q", "k"):
                            dst = SQT if sec == "q" else SKT
                            scl = 1.0 if sec == "q" else ML_DQK ** -0.5
                            for hh in range(4):
                                h = wt * 4 + hh
                                ps = self.psa[ev % 4]
                                self.b_style(xT, w, ws, hh * 128, ps, ntok, gt, "w")
                                s = ev % 4
                                self.evac(ev, sg[:, s, 0:ntok], ps[:, 0:ntok], [("pa", ev % 4)], [("stg", s)], scale=scl)
                                ev += 1
                                self.dma("sp", dst[h, :, tok0:tok0 + ntok], sg[:, s, 0:ntok], reads=[("stg", s)])
                        if sec in ("k", "v", "o", "g"):
                            for li, i in enumerate(gt):
                                t0, r = self.tiles[i]
                                ps = self.psa[ev % 4]
                                self.a_style(xT, w, ws, li, r, ps, ncols, "w")
                                s = ev % 4
                                if sec == "g":
                                    s2 = ev % 2
                                    self.evac(1, sgf[:r, s2, 0:2 * nh], ps[:r, 0:2 * nh], [("pa", ev % 4)], [("stgf", s2)])
                                    self.dma("sp", SG[t0:t0 + r, 0:2 * nh], sgf[:r, s2, 0:2 * nh], reads=[("stgf", s2)])
                                else:
                                    if sec == "k":
                                        self.evac(ev, sg[:r, s, :], ps[:r, :], [("pa", ev % 4)], [("stg", s)], scale=ML_DQK ** -0.5)
                                        d2 = SK[t0:t0 + r, wt * 512:(wt + 1) * 512]
                                    elif sec == "v":
                                        self.evac(ev, sg[:r, s, :], ps[:r, :], [("pa", ev % 4)], [("stg", s)])
                                        d2 = SV[t0:t0 + r, wt * 512:(wt + 1) * 512]
                                    else:
                                        self.evac(ev, sg[:r, s, :], ps[:r, :], [("pa", ev % 4)], [("stg", s)], func=AF.Sigmoid)
                                        d2 = SO[t0:t0 + r, wt * 512:(wt + 1) * 512]
                                    self.dma("sp", d2, sg[:r, s, :], reads=[("stg", s)])
                                ev += 1
        self.p.barrier()
        with contextlib.ExitStack() as st:
            G = self.sb(st, "G", [128, ntile, 2 * nh], F32)
            bi = self.bcast_row(st, "bi", t["ml_b_i"][j:j + 1, :], nh)
            bf = self.bcast_row(st, "bf", t["ml_b_f"][j:j + 1, :], nh)
            hg = self.bcast_row(st, "hg", t["ml_h_gain"][j:j + 1, :], VW)
            TI = self.sb(st, "TI", [128, ntile, nh], F32)
            TF = self.sb(st, "TF", [128, ntile, nh], F32)
            WS = self.sb(st, "WS", [128, ntile, nh], F32)
            ET = self.sb(st, "ET", [128, ntile, nh], F32)
            EG = self.sb(st, "EG", [128, ntile, nh], F32)
            self.op("dve", lambda e: e.memset(G[:, :, :], 0.0), writes=["G"])
            for i, (t0, r) in enumerate(self.tiles):
                self.dma("sp", G[:r, i, :], SG[t0:t0 + r, 0:2 * nh], writes=["G"])
            self.op("dve", lambda e: e.tensor_tensor(out=TI[:, :, :], in0=G[:, :, 0:nh], in1=bi[:, 0:nh].unsqueeze(1).broadcast_to([128, ntile, nh]), op=ALU.add), reads=["G", "bi"], writes=["TI"])
            self.op("dve", lambda e: e.tensor_tensor(out=TF[:, :, :], in0=G[:, :, nh:2 * nh], in1=bf[:, 0:nh].unsqueeze(1).broadcast_to([128, ntile, nh]), op=ALU.add), reads=["G", "bf"], writes=["TF"])
            self.op("act", lambda e: e.activation(out=TI[:, :, :], in_=TI[:, :, :], func=AF.Tanh, scale=1.0 / GATE_CAP), reads=["TI"], writes=["TI"])
            self.op("act", lambda e: e.activation(out=TF[:, :, :], in_=TF[:, :, :], func=AF.Tanh, scale=1.0 / GATE_CAP), reads=["TF"], writes=["TF"])
            self.op("act", lambda e: e.activation(out=TF[:, :, :], in_=TF[:, :, :], func=AF.Exp, scale=-GATE_CAP), reads=["TF"], writes=["TF"])
            self.op("act", lambda e: e.activation(out=TF[:, :, :], in_=TF[:, :, :], func=AF.Ln, bias=1.0), reads=["TF"], writes=["TF"])
            pnb, png = self.psa[0], self.psa[1]
            for i, (t0, r) in enumerate(self.tiles):
                self.op("pe", lambda e, i=i, r=r: e.matmul(pnb[:r, i * nh:(i + 1) * nh], lhsT=self.tri_f[:r, :r], rhs=TF[:r, i, :], start=True, stop=True), reads=["TF", "cf"], writes=[("pa", 0)])
                self.op("pe", lambda e, i=i, r=r: e.matmul(png[:, i * nh:(i + 1) * nh], lhsT=self.ones_f[:r, :], rhs=TF[:r, i, :], start=True, stop=True), reads=["TF", "cf"], writes=[("pa", 1)])
            nb3 = pnb[:, 0:ntile * nh].rearrange("p (i h) -> p i h", h=nh)
            ng3 = png[:, 0:ntile * nh].rearrange("p (i h) -> p i h", h=nh)
            self.op("dve", lambda e: e.scalar_tensor_tensor(out=WS[:, :, :], in0=TI[:, :, :], scalar=GATE_CAP, in1=nb3, op0=ALU.mult, op1=ALU.add), reads=["TI", ("pa", 0)], writes=["WS"])
            self.op("act", lambda e: e.activation(out=WS[:, :, :], in_=WS[:, :, :], func=AF.Exp), reads=["WS"], writes=["WS"])
            self.op("act", lambda e: e.activation(out=ET[:, :, :], in_=nb3, func=AF.Exp, scale=-1.0), reads=[("pa", 0)], writes=["ET"])
            self.op("act", lambda e: e.activation(out=EG[:, :, :], in_=ng3, func=AF.Exp, scale=-1.0), reads=[("pa", 1)], writes=["EG"])
            QT = self.sb(st, "QT", [128, 2, NT], BF16)
            KT = self.sb(st, "KT", [128, 2, NT], BF16)
            Kh = self.sb(st, "Kh", [128, 2, ntile, 128], BF16)
            Vh = self.sb(st, "Vh", [128, 2, ntile, 257], BF16)
            Oh = self.sb(st, "Oh", [128, 2, ntile, 256], BF16)
            Yh = self.sb(st, "Yh", [128, 2, ntile, 256], BF16)
            Z = self.sb(st, "Z", [128, 257], F32)
            Cb = self.sb(st, "Cb", [128, 2, 257], BF16)
            Sm = self.sb(st, "Sm", [128, 2, 128], BF16)
            Vp = self.sb(st, "Vp", [128, 2, 257], BF16)
            dd = self.sb(st, "dd", [128, 2, 8], F32)
            hh_ = self.sb(st, "hh", [128, 2, 256], F32)
            y1 = self.sb(st, "y1", [128, 2, 256], F32)
            jk = self.sb(st, "jk", [128, 256], BF16)
            self.op("dve", lambda e: e.memset(Vh[:, :, :, 256:257], 1.0), writes=["Vh"])
            cnt = 0
            for h in range(nh):
                hs_ = h % 2
                self.dma("sp", QT[:, hs_, :], SQT[h, :, :], writes=[("QT", hs_)])
                self.dma("sp", KT[:, hs_, :], SKT[h, :, :], writes=[("KT", hs_)])
                self.load_tiled(Kh, hs_, SK, h * 128, 128, "Kh")
                self.load_tiled(Vh, hs_, SV, h * 256, 256, "Vh")
                self.load_tiled(Oh, hs_, SO, h * 256, 256, "Oh")
                for i, (t0, r) in enumerate(self.tiles):
                    c = cnt % 2
                    cnt += 1
                    ps_s, ps_x, ps_c = self.psa[c], self.psa[2 + c], self.psa[4 + c]
                    self.op("pe", lambda e, hs_=hs_, t0=t0, r=r, ps_s=ps_s: e.matmul(ps_s[:r, 0:r], lhsT=KT[:, hs_, t0:t0 + r], rhs=QT[:, hs_, t0:t0 + r], start=True, stop=True),
                            reads=[("KT", hs_), ("QT", hs_)], writes=[("pa", c)])
                    self.op("dve", lambda e, r=r, c=c, ps_s=ps_s: e.tensor_tensor(out=Sm[:r, c, 0:r], in0=ps_s[:r, 0:r], in1=self.mask_bf[:r, 0:r], op=ALU.mult),
                            reads=[("pa", c), "mask_bf"], writes=[("Sm", c)])
                    self.op("act", lambda e, r=r, c=c, hs_=hs_, i=i, h=h: e.activation(out=Vp[:r, c, :], in_=Vh[:r, hs_, i, :], func=AF.Copy, scale=WS[:r, i, h:h + 1]),
                            reads=[("Vh", hs_), "WS"], writes=[("Vp", c)])
                    self.op("pe", lambda e, r=r, c=c, ps_x=ps_x, i=i: e.matmul(ps_x[:r, 0:257], lhsT=Sm[:r, c, 0:r], rhs=Vp[:r, c, :], start=True, stop=(i == 0)),
                            reads=[("Sm", c), ("Vp", c)], writes=[("pa", 2 + c)])
                    if i > 0:
                        cb = (i - 1) % 2
                        self.op("pe", lambda e, r=r, ps_x=ps_x, hs_=hs_, t0=t0, cb=cb: e.matmul(ps_x[:r, 0:257], lhsT=QT[:, hs_, t0:t0 + r], rhs=Cb[:, cb, :], start=False, stop=True),
                                reads=[("QT", hs_), ("Cb", cb)], writes=[("pa", 2 + c)])
                    if i < ntile - 1:
                        self.op("pe", lambda e, r=r, c=c, ps_c=ps_c, hs_=hs_, i=i: e.matmul(ps_c[:, 0:257], lhsT=Kh[:r, hs_, i, :], rhs=Vp[:r, c, :], start=True, stop=True),
                                reads=[("Kh", hs_), ("Vp", c)], writes=[("pa", 4 + c)])
                        if i == 0:
                            self.op("dve", lambda e, ps_c=ps_c: e.tensor_copy(out=Z[:, :], in_=ps_c[:, 0:257]), reads=[("pa", 4 + c)], writes=["Z"])
                        else:
                            self.op("dve", lambda e, ps_c=ps_c, i=i, h=h: e.scalar_tensor_tensor(out=Z[:, :], in0=Z[:, :], scalar=EG[:, i - 1, h:h + 1], in1=ps_c[:, 0:257], op0=ALU.mult, op1=ALU.add),
                                    reads=[("pa", 4 + c), "Z", "EG"], writes=["Z"])
                        self.op("act", lambda e, i=i, h=h: e.activation(out=Cb[:, i % 2, :], in_=Z[:, :], func=AF.Copy, scale=EG[:, i, h:h + 1]),
                                reads=["Z", "EG"], writes=[("Cb", i % 2)])
                    tk = [("dd", c)]
                    self.op("act", lambda e, r=r, c=c, ps_x=ps_x, i=i, h=h: e.activation(out=dd[:r, c, 0:1], in_=ps_x[:r, 256:257], func=AF.Abs, scale=ET[:r, i, h:h + 1]),
                            reads=[("pa", 2 + c), "ET"], writes=tk)
                    self.op("dve", lambda e, r=r, c=c: e.tensor_scalar(out=dd[:r, c, 1:2], in0=dd[:r, c, 0:1], scalar1=1.0, scalar2=None, op0=ALU.max), reads=tk, writes=tk)
                    self.op("dve", lambda e, r=r, c=c: e.reciprocal(out=dd[:r, c, 2:3], in_=dd[:r, c, 1:2]), reads=tk, writes=tk)
                    self.op("dve", lambda e, r=r, c=c, i=i, h=h: e.tensor_tensor(out=dd[:r, c, 3:4], in0=dd[:r, c, 2:3], in1=ET[:r, i, h:h + 1], op=ALU.mult), reads=tk + ["ET"], writes=tk)
                    self.op("act", lambda e, r=r, c=c, ps_x=ps_x: e.activation(out=hh_[:r, c, :], in_=ps_x[:r, 0:256], func=AF.Copy, scale=dd[:r, c, 3:4]),
                            reads=[("pa", 2 + c)] + tk, writes=[("hh", c)])
                    self.op("act", lambda e, r=r, c=c: e.activation(out=jk[:r, :], in_=hh_[:r, c, :], func=AF.Square, accum_out=dd[:r, c, 4:5]),
                            reads=[("hh", c)], writes=["jk"] + tk)
                    self.rstd(dd[:r, c, 4:5], dd[:r, c, 5:6], dd[:r, c, 6:7], 1.0 / ML_DV, tk)
                    self.op("dve", lambda e, r=r, c=c, h=h: e.scalar_tensor_tensor(out=y1[:r, c, :], in0=hh_[:r, c, :], scalar=dd[:r, c, 6:7], in1=hg[:r, h * 256:(h + 1) * 256], op0=ALU.mult, op1=ALU.mult),
                            reads=[("hh", c), "hg"] + tk, writes=[("y1", c)])
                    self.op("dve", lambda e, r=r, c=c, hs_=hs_, i=i: e.tensor_tensor(out=Yh[:r, hs_, i, :], in0=y1[:r, c, :], in1=Oh[:r, hs_, i, :], op=ALU.mult),
                            reads=[("y1", c), ("Oh", hs_)], writes=[("Yh", hs_)])
                self.store_tiled(SY, h * 256, 256, Yh, hs_, "Yh")
        self.p.barrier()
        self.out_proj(t["ml_w_out"][j])

    def L_fox(self, j):
        t = self.t
        self.mixer_begin()
        NT, ntile = self.NT, self.ntile
        HS = self.HS
        FW = D // HS
        FH = FOX_H // HS
        nkc = KC // HS
        Win = t["fox_w_in"][j].rearrange("(kc p) f -> p kc f", p=128)
        SQT, SKT, SV, SO, SG, SY = t["S_QT"], t["S_KT"], t["S_V"], t["S_O"], t["S_G"], t["S_Y"]
        GT = self.GP * 128
        with contextlib.ExitStack() as st:
            gam = self.bcast_row(st, "gam", t["fox_norm"][j:j + 1, :], D)
            gq = self.bcast_row(st, "gq", t["fox_q_gain"][j:j + 1, :], 64)
            gk = self.bcast_row(st, "gk", t["fox_k_gain"][j:j + 1, :], 64)
            bfb = self.bcast_row(st, "bfb", t["fox_b_f"][j:j + 1, :], FH)
            self.op("dve", lambda e: e.tensor_scalar(out=gq[:, :], in0=gq[:, :], scalar1=FOX_DH ** -0.5, scalar2=None, op0=ALU.mult), reads=["gq"], writes=["gq"])
            bufs = {"hs": self.sb(st, "hs", [128, 2, D], F32), "xn": self.sb(st, "xn", [128, 2, D], BF16),
                    "ss": self.sb(st, "ss", [128, 2, 4], F32), "junk": self.sb(st, "junk", [128, D], BF16)}
            xT = self.sb(st, "xT", [128, KC, GT], BF16)
            w = self.sb(st, "w", [128, 3, KC, 512], BF16)
            sg = self.sb(st, "stg", [128, 4, 512], BF16)
            sq = self.sb(st, "sq", [128, 2, 512], F32)
            qn = self.sb(st, "qn", [128, 2, 512], F32)
            rs = self.sb(st, "rs", [128, 2, 24], F32)
            qrow = self.sb(st, "qrow", [128, 2 * self.GP, FW], BF16)
            QTs = self.sb(st, "QTs", [128, nkc, GT], BF16)
            lfs = self.sb(st, "lfs", [128, 2, FH], F32)
            it = 0
            ev = 0
            for gt, tok0, ntok in self.groups():
                for li, i in enumerate(gt):
                    self.load_norm_T(bufs, i, gam, xT, li * 128, it)
                    it += 1
                for sec, ntl in (("q", 4 // HS), ("k", 4 // HS), ("v", 4 // HS), ("o", 4 // HS), ("f", 1)):
                    base = {"q": 0, "k": FW, "v": 2 * FW, "o": 3 * FW, "f": 4 * FW}[sec]
                    for wt in range(ntl):
                        ncols = FH if sec == "f" else 512
                        ws = self.wload(w, 3, Win[:, :, base + wt * 512:base + wt * 512 + ncols], ncols, "w")
                        for li, i in enumerate(gt):
                            t0, r = self.tiles[i]
                            pi = ev % 4
                            ps = self.psa[pi]
                            self.a_style(xT, w, ws, li, r, ps, ncols, "w")
                            s = ev % 4
                            s2 = ev % 2
                            if sec in ("q", "k"):
                                gn = gq if sec == "q" else gk
                                qi_ = li + (0 if sec == "q" else self.GP)
                                tk = [("rs", s2)]
                                self.op("act", lambda e, r=r, s2=s2, ps=ps: e.activation(out=sq[:r, s2, :], in_=ps[:r, :], func=AF.Square), reads=[("pa", pi)], writes=[("sq", s2)])
                                self.op("dve", lambda e, r=r, s2=s2: e.tensor_reduce(out=rs[:r, s2, 0:8], in_=sq[:r, s2, :].rearrange("p (h d) -> p h d", d=64), axis=AX.X, op=ALU.add), reads=[("sq", s2)], writes=tk)
                                self.rstd(rs[:r, s2, 0:8], rs[:r, s2, 8:16], rs[:r, s2, 16:24], 1.0 / FOX_DH, tk)
                                self.op("dve", lambda e, r=r, s2=s2, ps=ps: e.tensor_tensor(out=qn[:r, s2, :].rearrange("p (h d) -> p h d", d=64), in0=ps[:r, :].rearrange("p (h d) -> p h d", d=64),
                                                                                             in1=rs[:r, s2, 16:24].unsqueeze(2).broadcast_to([r, 8, 64]), op=ALU.mult),
                                        reads=[("pa", pi)] + tk, writes=[("qn", s2)])
                                self.op("dve", lambda e, r=r, s2=s2, gn=gn, qi_=qi_, wt=wt: e.tensor_tensor(out=qrow[:r, qi_, wt * 512:(wt + 1) * 512].rearrange("p (h d) -> p h d", d=64), in0=qn[:r, s2, :].rearrange("p (h d) -> p h d", d=64),
                                                                                                                 in1=gn[:r, 0:64].unsqueeze(1).broadcast_to([r, 8, 64]), op=ALU.mult),
                                        reads=[("qn", s2), self.ln(gn)], writes=[("qrow", qi_)])
                            elif sec == "v":
                                self.evac(ev, sg[:r, s, :], ps[:r, :], [("pa", pi)], [("stg", s)])
                                self.dma("sp", SV[t0:t0 + r, wt * 512:(wt + 1) * 512], sg[:r, s, :], reads=[("stg", s)])
                            elif sec == "o":
                                self.evac(ev, sg[:r, s, :], ps[:r, :], [("pa", pi)], [("stg", s)], func=AF.Sigmoid)
                                self.dma("sp", SO[t0:t0 + r, wt * 512:(wt + 1) * 512], sg[:r, s, :], reads=[("stg", s)])
                            else:
                                tk = [("lfs", s2)]
                                self.op("dve", lambda e, r=r, s2=s2, ps=ps: e.tensor_tensor(out=lfs[:r, s2, :], in0=ps[:r, 0:FH], in1=bfb[:r, :], op=ALU.add), reads=[("pa", pi), "bfb"], writes=tk)
                                self.op("act", lambda e, r=r, s2=s2: e.activation(out=lfs[:r, s2, :], in_=lfs[:r, s2, :], func=AF.Exp, scale=-1.0), reads=tk, writes=tk)
                                self.op("act", lambda e, r=r, s2=s2: e.activation(out=lfs[:r, s2, :], in_=lfs[:r, s2, :], func=AF.Ln, bias=1.0), reads=tk, writes=tk)
                                self.dma("sp", SG[t0:t0 + r, 0:FH], lfs[:r, s2, :], reads=tk)
                            ev += 1
                    if sec in ("q", "k"):
                        dst = SQT if sec == "q" else SKT
                        for li, i in enumerate(gt):
                            r = self.tiles[i][1]
                            qi_ = li + (0 if sec == "q" else self.GP)
                            self.transpose_into(qrow, qi_, r, QTs, li * 128, "qrow", nch=nkc)
                        self.dma("sp", dst[0:nkc, :, tok0:tok0 + ntok].rearrange("k p t -> p k t"), QTs[:, :, 0:ntok], reads=["QTs"])
        self.p.barrier()
        with contextlib.ExitStack() as st:
            G = self.sb(st, "G", [128, ntile, FH], F32)
            NC = self.sb(st, "NC", [128, ntile, FH], F32)
            NR = self.sb(st, "NR", [128, ntile, FH], F32)
            carry = self.sb(st, "carry", [128, FH], F32)
            self.op("dve", lambda e: e.memset(G[:, :, :], 0.0), writes=["G"])
            self.op("dve", lambda e: e.memset(carry[:, :], 0.0), writes=["carry"])
            for i, (t0, r) in enumerate(self.tiles):
                self.dma("sp", G[:r, i, :], SG[t0:t0 + r, 0:FH], writes=["G"])
            for i, (t0, r) in enumerate(self.tiles):
                p1, p2, p3 = self.psa[(i % 2) * 3], self.psa[(i % 2) * 3 + 1], self.psa[(i % 2) * 3 + 2]
                b = (i % 2) * 3
                self.op("pe", lambda e, i=i, r=r, p1=p1: e.matmul(p1[:r, 0:FH], lhsT=self.tri_f[:r, :r], rhs=G[:r, i, :], start=True, stop=True), reads=["G", "cf"], writes=[("pa", b)])
                self.op("pe", lambda e, i=i, r=r, p2=p2: e.matmul(p2[:, 0:FH], lhsT=self.ones_f[:r, :], rhs=G[:r, i, :], start=True, stop=True), reads=["G", "cf"], writes=[("pa", b + 1)])
                self.op("dve", lambda e, i=i, r=r, p1=p1: e.tensor_tensor(out=NC[:r, i, :], in0=p1[:r, 0:FH], in1=carry[:r, :], op=ALU.add), reads=[("pa", b), "carry"], writes=[("NC", i)])
                self.op("dve", lambda e, p2=p2: e.tensor_tensor(out=carry[:, :], in0=p2[:, 0:FH], in1=carry[:, :], op=ALU.add), reads=[("pa", b + 1), "carry"], writes=["carry"])
                sel = self.sel64 if r == 128 else self.sel8
                self.op("pe", lambda e, i=i, r=r, p3=p3, sel=sel: e.matmul(p3[:, 0:FH], lhsT=sel[:r, :], rhs=NC[:r, i, :], start=True, stop=True), reads=[("NC", i), "cf"], writes=[("pa", b + 2)])
                self.op("act", lambda e, i=i, p3=p3: e.copy(out=NR[:, i, :], in_=p3[:, 0:FH]), reads=[("pa", b + 2)], writes=[("NR", i)])
            QT = self.sb(st, "QT", [128, 2, NT], BF16)
            KT = self.sb(st, "KT", [128, 2, NT], BF16)
            Vp = self.sb(st, "Vp", [128, 2, ntile, 2, 65], BF16)
            Op = self.sb(st, "Op", [128, 2, ntile, 128], BF16)
            Yp = self.sb(st, "Yp", [128, 2, ntile, 128], BF16)
            bias = self.sb(st, "bias", [128, 2, ntile, 2], F32)
            P = self.sb(st, "P", [128, 4, 512], BF16)
            rd = self.sb(st, "rd", [128, 4, 2], F32)
            self.op("dve", lambda e: e.memset(Vp[:, :, :, :, 64:65], 1.0), writes=["Vp"])
            nf = self.nfull
            scnt = 0
            bcnt = 0
            for p in range(16 // HS):
                ps_ = p % 2
                self.dma("sp", QT[:, ps_, :], SQT[p, :, :], writes=[("QT", ps_)])
                self.dma("sp", KT[:, ps_, :], SKT[p, :, :], writes=[("KT", ps_)])
                if nf:
                    for hh in range(2):
                        self.dma("sp", Vp[:, ps_, 0:nf, hh, 0:64], SV[0:nf * 128, p * 128 + hh * 64:p * 128 + (hh + 1) * 64].rearrange("(i q) d -> q i d", q=128), writes=[("Vp", ps_)])
                if ntile > nf:
                    t0, r = self.tiles[nf]
                    self.dma("sp", Vp[:r, ps_, nf, :, 0:64], SV[t0:t0 + r, p * 128:(p + 1) * 128].rearrange("q (h d) -> q h d", h=2), writes=[("Vp", ps_)])
                self.load_tiled(Op, ps_, SO, p * 128, 128, "Op")
                for g0 in range(0, ntile, 4):
                    qis = list(range(g0, min(g0 + 4, ntile)))
                    q_end = self.tiles[qis[-1]][0] + self.tiles[qis[-1]][1]
                    for qi in qis:
                        pass
                    for kb in range(0, qis[-1] + 1):
                        k0, rk = self.tiles[kb]
                        qlo = max(kb, g0)
                        c0 = self.tiles[qlo][0]
                        N = q_end - c0
                        for hh in range(2):
                            sb_ = scnt % 4
                            scnt += 1
                            ps_s = self.psa[sb_]
                            self.op("pe", lambda e, hh=hh, ps_=ps_, k0=k0, rk=rk, c0=c0, N=N, ps_s=ps_s: e.matmul(ps_s[:rk, 0:N], lhsT=KT[hh * 64:(hh + 1) * 64, ps_, k0:k0 + rk], rhs=QT[hh * 64:(hh + 1) * 64, ps_, c0:c0 + N], start=True, stop=True),
                                    reads=[("KT", ps_), ("QT", ps_)], writes=[("pa", sb_)])
                            for qi in range(qlo, qis[-1] + 1):
                                tq0, rq = self.tiles[qi]
                                off = tq0 - c0
                                ql = qi - g0
                                bs = bcnt % 2
                                bcnt += 1
                                hcol = 2 * p + hh
                                self.op("dve", lambda e, rk=rk, bs=bs, kb=kb, qi=qi, hcol=hcol: e.tensor_tensor(out=bias[:rk, bs, 0, 0:1], in0=NC[:rk, kb, hcol:hcol + 1], in1=NR[:rk, qi, hcol:hcol + 1], op=ALU.subtract),
                                        reads=[("NC", kb), ("NR", qi)], writes=[("bias", bs)])
                                self.op("act", lambda e, rk=rk, rq=rq, off=off, sb_=sb_, ps_s=ps_s, bs=bs: e.activation(out=P[:rk, sb_, off:off + rq], in_=ps_s[:rk, off:off + rq], func=AF.Exp, bias=bias[:rk, bs, 0, 0:1]),
                                        reads=[("pa", sb_), ("bias", bs)], writes=[("P", sb_)])
                                if qi == kb:
                                    self.op("dve", lambda e, rk=rk, rq=rq, off=off, sb_=sb_: e.tensor_tensor(out=P[:rk, sb_, off:off + rq], in0=P[:rk, sb_, off:off + rq], in1=self.mask_bf[:rk, 0:rq], op=ALU.mult),
                                            reads=[("P", sb_), "mask_bf"], writes=[("P", sb_)])
                                po = self.psa[4 + hh]
                                self.op("pe", lambda e, rk=rk, rq=rq, off=off, sb_=sb_, po=po, ql=ql, kb=kb, qi=qi, hh=hh, ps_=ps_, g0=g0: e.matmul(po[:rq, ql * 65:(ql + 1) * 65], lhsT=P[:rk, sb_, off:off + rq], rhs=Vp[:rk, ps_, kb, hh, :], start=(kb == 0 and qi == g0), stop=(kb == qi)),
                                        reads=[("P", sb_), ("Vp", ps_)], writes=[("pa", 4 + hh)])
                    for qi in qis:
                        tq0, rq = self.tiles[qi]
                        ql = qi - g0
                        for hh in range(2):
                            po = self.psa[4 + hh]
                            self.op("dve", lambda e, rq=rq, ql=ql, hh=hh, po=po: e.reciprocal(out=rd[:rq, ql, hh:hh + 1], in_=po[:rq, ql * 65 + 64:ql * 65 + 65]), reads=[("pa", 4 + hh)], writes=[("rd", ql)])
                            self.op("dve", lambda e, rq=rq, ql=ql, hh=hh, po=po, qi=qi, ps_=ps_: e.scalar_tensor_tensor(out=Yp[:rq, ps_, qi, hh * 64:(hh + 1) * 64], in0=po[:rq, ql * 65:ql * 65 + 64], scalar=rd[:rq, ql, hh:hh + 1],
                                                                                                                        in1=Op[:rq, ps_, qi, hh * 64:(hh + 1) * 64], op0=ALU.mult, op1=ALU.mult),
                                    reads=[("pa", 4 + hh), ("rd", ql), ("Op", ps_)], writes=[("Yp", ps_)])
                self.store_tiled(SY, p * 128, 128, Yp, ps_, "Yp")
        self.p.barrier()
        self.out_proj(t["fox_w_out"][j])

    def L_final(self):
        t = self.t
        H, out = t["H"], t["out"]
        with contextlib.ExitStack() as st:
            gam = self.bcast_row(st, "gamf", t["final_norm"][0:1, :], D)
            hs = self.sb(st, "hsf", [128, 2, D], F32)
            ot = self.sb(st, "otf", [128, 2, D], F32)
            ss = self.sb(st, "ssf", [128, 2, 4], F32)
            junk = self.sb(st, "junkf", [128, D], BF16)
            for i, (t0, r) in enumerate(self.tiles):
                s = i % 2
                self.dma("sp", hs[:r, s, :], H[t0:t0 + r, :], reads=[("H", i)], writes=[("hsf", s)])
                self.op("act", lambda e, r=r, s=s: e.activation(out=junk[:r, :], in_=hs[:r, s, :], func=AF.Square, accum_out=ss[:r, s, 0:1]),
                        reads=[("hsf", s)], writes=["junkf", ("ssf", s)])
                self.rstd(ss[:r, s, 0:1], ss[:r, s, 1:2], ss[:r, s, 2:3], 1.0 / D, [("ssf", s)])
                self.op("dve", lambda e, r=r, s=s: e.scalar_tensor_tensor(out=ot[:r, s, :], in0=hs[:r, s, :], scalar=ss[:r, s, 2:3], in1=gam[:r, :], op0=ALU.mult, op1=ALU.mult),
                        reads=[("hsf", s), ("ssf", s), "gamf"], writes=[("otf", s)])
                if self.split:
                    self.dma("sp", out[t0:t0 + r, :], ot[0:r, s, :], reads=[("otf", s)])
                    continue
                lo = N_META if i == 0 else 0
                self.dma("sp", out[t0 + lo - N_META:t0 + r - N_META, :], ot[lo:r, s, :], reads=[("otf", s)])


def build_nc(NT, layers, final=True, split=False, rgroups=None):
    nc = bass.Bass("TRN2", target_bir_lowering=False)
    kb = KB(nc, NT, layers, final, split, rgroups)
    kb.build()
    return nc, kb


def make_consts():
    c = np.zeros((128, 640), np.float32)
    c[:, 0:128] = np.eye(128)
    c[:, 128:256] = np.triu(np.ones((128, 128)))
    c[:, 256:384] = 1.0
    c[64, 384:512] = 1.0
    c[8, 512:640] = 1.0
    return c


ALL_LAYERS = [("ml", 0), ("ffn", 0), ("fox", 0), ("moe", 0), ("ml", 1), ("ffn", 1), ("fox", 1), ("moe", 1)]
NCORES = 8


def shard_weights(inputs, r, names):
    out = {}
    for name in names:
        if name in ("x", "consts"):
            continue
        a = inputs[name]
        if name == "ml_w_in":
            q = a[:, :, r * 512:(r + 1) * 512]
            k = a[:, :, 1024 + r * 512:1024 + (r + 1) * 512]
            v = a[:, :, 2048 + r * 1024:2048 + (r + 1) * 1024]
            o = a[:, :, 4096 + r * 1024:4096 + (r + 1) * 1024]
            gi = a[:, :, 6144 + 4 * r:6144 + 4 * r + 4]
            gf = a[:, :, 6152 + 4 * r:6152 + 4 * r + 4]
            a = np.concatenate([q, k, v, o, gi, gf], axis=2)
        elif name in ("ml_b_i", "ml_b_f"):
            a = a[:, 4 * r:4 * r + 4]
        elif name == "ml_h_gain":
            a = a[:, r * 1024:(r + 1) * 1024]
        elif name in ("ml_w_out", "fox_w_out"):
            a = a[:, r * 1024:(r + 1) * 1024, :]
        elif name == "fox_w_in":
            a = np.concatenate([a[:, :, c + r * 1024:c + (r + 1) * 1024] for c in (0, 2048, 4096, 6144)] + [a[:, :, 8192 + 16 * r:8192 + 16 * r + 16]], axis=2)
        elif name == "fox_b_f":
            a = a[:, 16 * r:16 * r + 16]
        elif name == "final_norm":
            a = np.asarray(a).reshape(1, D)
        out[name] = np.ascontiguousarray(a, dtype=np.float32)
    return out


def kernel(**inputs):
    B, S, _ = inputs["x"].shape
    nc, kb = build_nc(S + N_META, ALL_LAYERS)
    shared = {}
    for name in kb.used_inputs:
        if name == "x":
            continue
        if name == "consts":
            shared[name] = make_consts()
        elif name == "final_norm":
            shared[name] = np.ascontiguousarray(inputs[name], dtype=np.float32).reshape(1, D)
        else:
            shared[name] = np.ascontiguousarray(inputs[name], dtype=np.float32)
    x = np.ascontiguousarray(inputs["x"], dtype=np.float32)
    in_maps = []
    for b in range(B):
        m = dict(shared)
        m["x"] = x[b]
        in_maps.append(m)
    res = run_bass_kernel_spmd(nc, in_maps, core_ids=list(range(B)))
    return np.stack([res.results[b]["out"] for b in range(B)], axis=0).astype(np.float32)
```
